# Optimizing a Trainium2 kernel written in Bass

```python
import math
import jax
import jax.numpy as jnp
from jax import lax
import numpy as np

D_MODEL = 1024
BATCH = 8
SEQ = 4096
DEPTH = 2

MEM_LEN = 256
D_RG = 1024
RG_BLOCKS = 4
RG_BLOCK = D_RG // RG_BLOCKS
RG_C = 8.0
CONV_W = 4
D_ML = 1024
ML_HEADS = 4
ML_HEAD_DIM = D_ML // ML_HEADS
ML_CHUNK = 128
D_XA = 1024
XA_HEADS = 4
XA_HEAD_DIM = D_XA // XA_HEADS
N_BRANCH = 3
N_IN = 2 * D_RG + 2 * D_ML + 2 * ML_HEADS + D_XA + N_BRANCH * D_MODEL
D_FF = 3584
N_EXPERTS = 8
TOP_K = 2
MOE_BLOCK = 128
N_DENSE = (DEPTH + 1) // 2
N_MOE = DEPTH // 2
EPS = 1e-6

kernel_name = "hybrid_rglru_mlstm_memxattn_moe"


def rms_norm(x, g):
    xf = x.astype(jnp.float32)
    y = xf * lax.rsqrt(jnp.mean(xf * xf, axis=-1, keepdims=True) + EPS)
    return (y * g.astype(jnp.float32)).astype(x.dtype)


def causal_dwconv(x, w, b):
    C = x.shape[-1]
    y = lax.conv_general_dilated(
        x, w[:, None, :].astype(x.dtype), window_strides=(1,),
        padding=[(w.shape[0] - 1, 0)], dimension_numbers=("NWC", "WIO", "NWC"),
        feature_group_count=C)
    return y + b


def block_diag(x, w):
    G, bi, bo = w.shape
    xs = x.reshape(*x.shape[:-1], G, bi)
    return jnp.einsum("...gi,gio->...go", xs, w).reshape(*x.shape[:-1], G * bo)


def split_columns(z):
    sizes = [D_RG, D_RG, D_ML, D_ML, ML_HEADS, ML_HEADS, D_XA, N_BRANCH * D_MODEL]
    out = []
    off = 0
    for s in sizes:
        out.append(z[..., off:off + s])
        off += s
    return out


def rg_lru(x, w_a, b_a, w_x, b_x, lam):
    xf = x.astype(jnp.float32)
    r = jax.nn.sigmoid(block_diag(x, w_a).astype(jnp.float32) + b_a.astype(jnp.float32))
    i = jax.nn.sigmoid(block_diag(x, w_x).astype(jnp.float32) + b_x.astype(jnp.float32))
    log_a = -RG_C * r * jax.nn.softplus(-lam.astype(jnp.float32))
    a = jnp.exp(log_a)
    b_term = jnp.sqrt(-jnp.expm1(2.0 * log_a)) * (i * xf)

    def combine(left, right):
        a1, b1 = left
        a2, b2 = right
        return a1 * a2, a2 * b1 + b2

    _, h = lax.associative_scan(combine, (a, b_term), axis=1)
    return h.astype(x.dtype)


def mlstm_chunkwise(q, k, v, i_pre, f_pre):
    B, S, H, d = q.shape
    L = ML_CHUNK
    NC = S // L

    def to_chunks(t):
        t = t.reshape(B, NC, L, H, *t.shape[3:])
        return jnp.moveaxis(t, (1, 3), (0, 2))

    qc, kc, vc = to_chunks(q), to_chunks(k), to_chunks(v)
    ic = to_chunks(i_pre)
    lfc = to_chunks(jax.nn.log_sigmoid(f_pre))
    causal = jnp.tril(jnp.ones((L, L), dtype=bool))

    def step(carry, inp):
        C, n, m = carry
        qb, kb, vb, ib, lf = inp
        b = jnp.cumsum(lf, axis=-1)
        g = b + m[..., None]
        D = b[..., :, None] - b[..., None, :] + ib[..., None, :]
        D = jnp.where(causal, D, -jnp.inf)
        m_row = jnp.maximum(g, jnp.max(D, axis=-1))
        W = jnp.exp(D - m_row[..., None]) * jnp.einsum("bhjd,bhsd->bhjs", qb, kb)
        inter = jnp.exp(g - m_row)
        num = inter[..., None] * jnp.einsum("bhjk,bhkv->bhjv", qb, C) + jnp.einsum("bhjs,bhsv->bhjv", W, vb)
        den = inter * jnp.einsum("bhjk,bhk->bhj", qb, n) + jnp.sum(W, axis=-1)
        h = num / jnp.maximum(jnp.abs(den), jnp.exp(-m_row))[..., None]
        bL = b[..., -1]
        dL = bL[..., None] - b + ib
        m_new = jnp.maximum(bL + m, jnp.max(dL, axis=-1))
        decay = jnp.exp(bL + m - m_new)
        wL = jnp.exp(dL - m_new[..., None])
        C_new = decay[..., None, None] * C + jnp.einsum("bhs,bhsk,bhsv->bhkv", wL, kb, vb)
        n_new = decay[..., None] * n + jnp.einsum("bhs,bhsk->bhk", wL, kb)
        return (C_new, n_new, m_new), h

    init = (jnp.zeros((B, H, d, d), jnp.float32), jnp.zeros((B, H, d), jnp.float32),
            jnp.zeros((B, H), jnp.float32))
    _, hc = lax.scan(step, init, (qc, kc, vc, ic, lfc))
    return jnp.moveaxis(hc, (0, 2), (1, 3)).reshape(B, S, H, d)


def mlstm_branch(u, o_pre, i_pre, f_pre, conv_w, conv_b, w_q, w_k, w_v, b_i, b_f, norm_g):
    B, S, _ = u.shape
    c = jax.nn.silu(causal_dwconv(u, conv_w, conv_b))
    q = block_diag(c, w_q).reshape(B, S, ML_HEADS, ML_HEAD_DIM).astype(jnp.float32)
    k = block_diag(c, w_k).reshape(B, S, ML_HEADS, ML_HEAD_DIM).astype(jnp.float32) * (ML_HEAD_DIM ** -0.5)
    v = block_diag(u, w_v).reshape(B, S, ML_HEADS, ML_HEAD_DIM).astype(jnp.float32)
    ig = i_pre.astype(jnp.float32) + b_i.astype(jnp.float32)
    fg = f_pre.astype(jnp.float32) + b_f.astype(jnp.float32)
    h = mlstm_chunkwise(q, k, v, ig, fg)
    o = jax.nn.sigmoid(o_pre.astype(jnp.float32)).reshape(B, S, ML_HEADS, ML_HEAD_DIM)
    h = o * h
    h = h * lax.rsqrt(jnp.mean(h * h, axis=-1, keepdims=True) + EPS)
    return (h.reshape(B, S, D_ML) * norm_g.astype(jnp.float32)).astype(u.dtype)


def memory_attention(q, mem_n, w_kv):
    B, S, _ = q.shape
    M = mem_n.shape[1]
    kv = (mem_n @ w_kv).reshape(B, M, 2, XA_HEADS, XA_HEAD_DIM)
    k, v = kv[:, :, 0], kv[:, :, 1]
    qh = q.reshape(B, S, XA_HEADS, XA_HEAD_DIM)
    s = jnp.einsum("bshd,bmhd->bhsm", qh, k).astype(jnp.float32) * (XA_HEAD_DIM ** -0.5)
    p = jax.nn.softmax(s, axis=-1).astype(q.dtype)
    o = jnp.einsum("bhsm,bmhd->bshd", p, v)
    return o.reshape(B, S, D_XA)


def swiglu(x, w1, w3, w2):
    return (jax.nn.silu(x @ w1) * (x @ w3)) @ w2


def moe_swiglu(x2, router_w, router_b, w1, w3, w2):
    T, D = x2.shape
    logits = (x2 @ router_w).astype(jnp.float32) + router_b.astype(jnp.float32)
    top_val, top_idx = lax.top_k(logits, TOP_K)
    gates = jax.nn.softmax(top_val, axis=-1).astype(x2.dtype)
    A = T * TOP_K
    e_flat = top_idx.reshape(A).astype(jnp.int32)
    g_flat = gates.reshape(A)
    tok_flat = jnp.arange(A, dtype=jnp.int32) // TOP_K
    order = jnp.argsort(e_flat)
    e_sorted = e_flat[order]
    tok_sorted = tok_flat[order]
    g_sorted = g_flat[order]
    counts = jnp.bincount(e_flat, length=N_EXPERTS).astype(jnp.int32)
    padded = ((counts + MOE_BLOCK - 1) // MOE_BLOCK) * MOE_BLOCK
    start = jnp.cumsum(counts) - counts
    pend = jnp.cumsum(padded)
    pstart = pend - padded
    dest = pstart[e_sorted] + (jnp.arange(A, dtype=jnp.int32) - start[e_sorted])
    P = A + N_EXPERTS * MOE_BLOCK
    NB = P // MOE_BLOCK
    buf_tok = jnp.zeros((P,), jnp.int32).at[dest].set(tok_sorted)
    buf_g = jnp.zeros((P,), x2.dtype).at[dest].set(g_sorted)
    blk_start = jnp.arange(NB, dtype=jnp.int32) * MOE_BLOCK
    blk_e = jnp.minimum(jnp.searchsorted(pend, blk_start, side="right"), N_EXPERTS - 1).astype(jnp.int32)

    def expert_block(args):
        tok_b, e_b = args
        xb = x2[tok_b]
        return swiglu(xb, w1[e_b], w3[e_b], w2[e_b])

    y_blk = lax.map(expert_block, (buf_tok.reshape(NB, MOE_BLOCK), blk_e))
    y = y_blk.reshape(P, D) * buf_g[:, None]
    return jnp.zeros((T, D), x2.dtype).at[buf_tok].add(y)


def setup_inputs(seed: int = 0) -> dict:
    key = jax.random.key(seed)
    ks = iter(jax.random.split(key, 48))
    f32 = jnp.float32

    def nrm(shape, scale):
        return jax.random.normal(next(ks), shape, f32) * scale

    def gain(shape):
        return 1.0 + nrm(shape, 0.05)

    res_scale = (2.0 * DEPTH) ** -0.5
    u = jax.random.uniform(next(ks), (DEPTH, D_RG), f32, minval=0.9, maxval=0.999)
    p = u ** (1.0 / RG_C)
    rg_lambda = jnp.log(p) - jnp.log1p(-p)
    ml_b_f = jnp.linspace(3.0, 6.0, ML_HEADS, dtype=f32)[None, :] + nrm((DEPTH, ML_HEADS), 0.1)
    return {
        "x": nrm((BATCH, SEQ, D_MODEL), 1.0),
        "mem": nrm((BATCH, MEM_LEN, D_MODEL), 1.0),
        "norm_mix_g": gain((DEPTH, D_MODEL)),
        "w_in": nrm((DEPTH, D_MODEL, N_IN), D_MODEL ** -0.5),
        "conv_rg_w": nrm((DEPTH, CONV_W, D_RG), CONV_W ** -0.5),
        "conv_rg_b": nrm((DEPTH, D_RG), 0.02),
        "rg_w_a": nrm((DEPTH, RG_BLOCKS, RG_BLOCK, RG_BLOCK), RG_BLOCK ** -0.5),
        "rg_b_a": nrm((DEPTH, D_RG), 0.1),
        "rg_w_x": nrm((DEPTH, RG_BLOCKS, RG_BLOCK, RG_BLOCK), RG_BLOCK ** -0.5),
        "rg_b_x": nrm((DEPTH, D_RG), 0.1),
        "rg_lambda": rg_lambda,
        "conv_ml_w": nrm((DEPTH, CONV_W, D_ML), CONV_W ** -0.5),
        "conv_ml_b": nrm((DEPTH, D_ML), 0.02),
        "ml_w_q": nrm((DEPTH, ML_HEADS, ML_HEAD_DIM, ML_HEAD_DIM), ML_HEAD_DIM ** -0.5),
        "ml_w_k": nrm((DEPTH, ML_HEADS, ML_HEAD_DIM, ML_HEAD_DIM), ML_HEAD_DIM ** -0.5),
        "ml_w_v": nrm((DEPTH, ML_HEADS, ML_HEAD_DIM, ML_HEAD_DIM), ML_HEAD_DIM ** -0.5),
        "ml_b_i": nrm((DEPTH, ML_HEADS), 0.1),
        "ml_b_f": ml_b_f,
        "ml_norm_g": gain((DEPTH, D_ML)),
        "mem_norm_g": gain((DEPTH, D_MODEL)),
        "w_kv": nrm((DEPTH, D_MODEL, 2 * D_XA), D_MODEL ** -0.5),
        "w_br_rg": nrm((DEPTH, D_RG, D_MODEL), D_RG ** -0.5),
        "w_br_ml": nrm((DEPTH, D_ML, D_MODEL), D_ML ** -0.5),
        "w_br_xa": nrm((DEPTH, D_XA, D_MODEL), D_XA ** -0.5),
        "b_merge": nrm((DEPTH, N_BRANCH * D_MODEL), 0.1),
        "w_out": nrm((DEPTH, D_MODEL, D_MODEL), D_MODEL ** -0.5 * res_scale),
        "norm_ffn_g": gain((DEPTH, D_MODEL)),
        "ffn_w1": nrm((N_DENSE, D_MODEL, D_FF), D_MODEL ** -0.5),
        "ffn_w3": nrm((N_DENSE, D_MODEL, D_FF), D_MODEL ** -0.5),
        "ffn_w2": nrm((N_DENSE, D_FF, D_MODEL), D_FF ** -0.5 * res_scale),
        "router_w": nrm((N_MOE, D_MODEL, N_EXPERTS), D_MODEL ** -0.5),
        "router_b": nrm((N_MOE, N_EXPERTS), 0.01),
        "moe_w1": nrm((N_MOE, N_EXPERTS, D_MODEL, D_FF), D_MODEL ** -0.5),
        "moe_w3": nrm((N_MOE, N_EXPERTS, D_MODEL, D_FF), D_MODEL ** -0.5),
        "moe_w2": nrm((N_MOE, N_EXPERTS, D_FF, D_MODEL), D_FF ** -0.5 * res_scale),
        "final_norm_g": gain((D_MODEL,)),
    }


def reference(x, mem, norm_mix_g, w_in, conv_rg_w, conv_rg_b, rg_w_a, rg_b_a, rg_w_x, rg_b_x,
              rg_lambda, conv_ml_w, conv_ml_b, ml_w_q, ml_w_k, ml_w_v, ml_b_i, ml_b_f, ml_norm_g,
              mem_norm_g, w_kv, w_br_rg, w_br_ml, w_br_xa, b_merge, w_out, norm_ffn_g,
              ffn_w1, ffn_w3, ffn_w2, router_w, router_b, moe_w1, moe_w3, moe_w2, final_norm_g):
    B, S, D = x.shape
    for l in range(DEPTH):
        h = rms_norm(x, norm_mix_g[l])
        z = h @ w_in[l]
        a_x, a_y, m_u, m_o, m_i, m_f, xa_q, gate_pre = split_columns(z)
        xa_c = causal_dwconv(a_x, conv_rg_w[l], conv_rg_b[l])
        y_rg = rg_lru(xa_c, rg_w_a[l], rg_b_a[l], rg_w_x[l], rg_b_x[l], rg_lambda[l]) * jax.nn.gelu(a_y)
        y_ml = mlstm_branch(m_u, m_o, m_i, m_f, conv_ml_w[l], conv_ml_b[l], ml_w_q[l], ml_w_k[l],
                            ml_w_v[l], ml_b_i[l], ml_b_f[l], ml_norm_g[l])
        mem_n = rms_norm(mem, mem_norm_g[l])
        y_xa = memory_attention(xa_q, mem_n, w_kv[l])
        gates = jax.nn.sigmoid((gate_pre + b_merge[l]).astype(jnp.float32)).astype(x.dtype)
        gates = gates.reshape(B, S, N_BRANCH, D)
        merged = (gates[:, :, 0] * (y_rg @ w_br_rg[l])
                  + gates[:, :, 1] * (y_ml @ w_br_ml[l])
                  + gates[:, :, 2] * (y_xa @ w_br_xa[l]))
        x = x + merged @ w_out[l]
        h2 = rms_norm(x, norm_ffn_g[l])
        j = l // 2
        if l % 2 == 0:
            f = swiglu(h2, ffn_w1[j], ffn_w3[j], ffn_w2[j])
        else:
            f = moe_swiglu(h2.reshape(B * S, D), router_w[j], router_b[j],
                           moe_w1[j], moe_w3[j], moe_w2[j]).reshape(B, S, D)
        x = x + f
    return rms_norm(x, final_norm_g)
```

```python
import numpy as np
from contextlib import ExitStack
import concourse.bass as bass
import concourse.mybir as mybir
from concourse.bass_utils import run_bass_kernel_spmd

F32 = mybir.dt.float32
BF16 = mybir.dt.bfloat16
AF = mybir.ActivationFunctionType
ALU = mybir.AluOpType
AX = mybir.AxisListType

D = 1024
KC = 8
T = 512
NCH = 4
MEM = 256
DFF = 3584
NF = 28
NEXP = 8
N_IN = 8200
EPS = 1e-6
SLOT = 2048
NBF = 8
NCVT = 8
NSTG = 3

PP_NAMES = [("g_mix", 8), ("g_ffn", 8), ("crw", 32), ("crb", 8), ("rba", 8), ("rbx", 8), ("lam", 8),
            ("cmw", 32), ("cmb", 8), ("gml", 8), ("gmem", 8), ("bmg", 24)]
PP_OFF = {}
_o = 0
for _n, _c in PP_NAMES:
    PP_OFF[_n] = _o
    _o += _c
PP_L = _o
BC_FINAL = 0
BC_RB = 1024
BC_BI = 1032


class V:
    __slots__ = ("ap", "keys")

    def __init__(self, ap, keys):
        self.ap = ap
        self.keys = tuple(keys)

    def __getitem__(self, idx):
        return V(self.ap[idx], self.keys)

    def re(self, pat, **kw):
        return V(self.ap.rearrange(pat, **kw), self.keys)

    def bc(self, shape):
        return V(self.ap.to_broadcast(shape), self.keys)

    def cast(self, dt):
        return V(self.ap.bitcast(dt), self.keys)


class Prog:
    ENG = ('pe', 'act', 'dve', 'pool', 'sp')

    def __init__(self, nc):
        self.nc = nc
        self.streams = {e: [] for e in self.ENG}
        self.count = {}
        self.last_w = {}
        self.readers = {}
        self.waited = {e: {} for e in self.ENG}
        self.semnames = list(self.ENG)
        self.deferred = None

    def _deps(self, eng, reads, writes):
        deps = []
        for k in reads:
            p = self.last_w.get(k)
            if p is not None:
                deps.append(p)
        for k in writes:
            p = self.last_w.get(k)
            if p is not None:
                deps.append(p)
            r = self.readers.get(k)
            if r:
                deps.extend(r.values())
        need = {}
        wd = self.waited[eng]
        for (s, v) in deps:
            if s == 'pe' and eng == 'pe':
                continue
            if wd.get(s, 0) >= v:
                continue
            if need.get(s, 0) < v:
                need[s] = v
        for s, v in need.items():
            wd[s] = v
            self.streams[eng].append(('wait', s, v))

    def _commit(self, prod, reads, writes):
        for k in reads:
            self.readers.setdefault(k, {})[prod[0]] = prod
        for k in writes:
            self.last_w[k] = prod
            self.readers[k] = {}

    def op(self, eng, fn, reads=(), writes=()):
        if self.deferred is not None:
            self.deferred.append((0, eng, fn, tuple(reads), tuple(writes)))
            return
        self._op(eng, fn, reads, writes)

    def dma(self, eng, chan, fn, reads=(), writes=()):
        if self.deferred is not None:
            self.deferred.append((1, eng, chan, fn, tuple(reads), tuple(writes)))
            return
        self._dma(eng, chan, fn, reads, writes)

    def begin_defer(self):
        self.deferred = []

    def end_defer(self):
        lst = self.deferred
        self.deferred = None
        return lst

    def feed(self, lst, pos, n):
        assert self.deferred is None
        end = min(len(lst), pos + n)
        for i in range(pos, end):
            r = lst[i]
            if r[0] == 0:
                self._op(r[1], r[2], r[3], r[4])
            else:
                self._dma(r[1], r[2], r[3], r[4], r[5])
        return end

    def _op(self, eng, fn, reads=(), writes=()):
        if eng != 'pe':
            ex = tuple(k for k in reads if isinstance(k, tuple) and k and k[0] == 'ps')
            if ex:
                writes = tuple(writes) + ex
        self._deps(eng, reads, writes)
        v = self.count.get(eng, 0) + 1
        self.count[eng] = v
        self.streams[eng].append(('op', fn, eng, 1))
        self._commit((eng, v), reads, writes)

    def _dma(self, eng, chan, fn, reads=(), writes=()):
        if chan not in self.semnames:
            self.semnames.append(chan)
        self._deps(eng, reads, writes)
        v = self.count.get(chan, 0) + 16
        self.count[chan] = v
        self.streams[eng].append(('op', fn, chan, 16))
        self._commit((chan, v), reads, writes)

    def wait_all(self, eng, keys):
        self._deps(eng, keys, ())

    def emit(self):
        nc = self.nc
        needed = {}
        for e in self.ENG:
            for ent in self.streams[e]:
                if ent[0] == 'wait':
                    needed.setdefault(ent[1], set()).add(ent[2])
        remap = {}
        for sname, vals in needed.items():
            if sname in self.ENG:
                remap[sname] = {v: i + 1 for i, v in enumerate(sorted(vals))}
        with ExitStack() as st:
            sems = {s: st.enter_context(nc.semaphore("s_" + "".join(ch if ch.isalnum() else "_" for ch in str(s)))) for s in self.semnames}
            block = st.enter_context(nc.Block())
            engobj = {'pe': 'tensor', 'act': 'scalar', 'dve': 'vector', 'pool': 'gpsimd', 'sp': 'sync'}

            def mk(e):
                def body(eo):
                    idx = 0
                    rm = remap.get(e, {})
                    for ent in self.streams[e]:
                        if ent[0] == 'wait':
                            v = ent[2]
                            if ent[1] in remap:
                                v = remap[ent[1]][v]
                            eo.wait_ge(sems[ent[1]], v)
                        else:
                            ins = ent[1](eo)
                            if ent[2] == e:
                                idx += 1
                                if idx in rm:
                                    ins.then_inc(sems[e], 1)
                            else:
                                ins.then_inc(sems[ent[2]], ent[3])
                return body
            for e in self.ENG:
                getattr(block, engobj[e])(mk(e))


def _keys(*vs):
    ks = ()
    for v in vs:
        if isinstance(v, V):
            ks += v.keys
    return ks


def _a(v):
    return v.ap if isinstance(v, V) else v


class K:
    def __init__(self, S, depth, moe_layers, stages="kv,norm,rg,ml,xa,ffn"):
        self.S, self.depth, self.moe_layers = S, depth, moe_layers
        self.stages = set(stages.split(","))
        self.NT = S // T
        nc = self.nc = bass.Bass("TRN2", target_bir_lowering=False)
        self.P = Prog(nc)
        self.st = ExitStack()
        self.din = {}
        self.BANKS = {'m': [6, 7], 'f': [0, 1, 2, 3, 4, 5]}
        self.SLOTS = {'m': [5, 6, 7], 'f': [0, 1, 2, 3, 4]}
        self.bank_pos = {'m': 0, 'f': 0}
        self.slot_pos = {'m': 0, 'f': 0}
        self.cur_pool = 'm'
        self.stg_i = 0
        self.pids = {}
        self.marks = []
        self.leftover = []

    def tick(self):
        lst = getattr(self, 'feed_lst', None)
        if not lst:
            return
        self.ticks_left -= 1
        if self.feed_delay > 0:
            self.feed_delay -= 1
            return
        remaining = len(lst) - self.feed_pos
        if remaining <= 0:
            return
        fair = remaining / max(1, self.ticks_left)
        quota = int(2.0 * fair) + 2
        min_n = max(1, int(fair + 0.999))
        written = {}
        n = 0
        while self.feed_pos < len(lst) and n < quota:
            r = lst[self.feed_pos]
            eng = r[1]
            reads = r[3] if r[0] == 0 else r[4]
            writes = r[4] if r[0] == 0 else r[5]
            if n >= min_n and any(written.get(k, eng) != eng for k in reads):
                break
            self.feed_pos = self.P.feed(lst, self.feed_pos, 1)
            n += 1
            for k in writes:
                written[k] = eng

    def mm(self, out, lhsT, rhs, start=True, stop=True):
        self.P.op('pe', lambda e: e.matmul(out.ap, lhsT.ap, rhs.ap, start=start, stop=stop),
                  reads=_keys(lhsT, rhs), writes=out.keys)

    def tr(self, out, in_, ident):
        self.P.op('pe', lambda e: e.transpose(out.ap, in_.ap, ident.ap), reads=_keys(in_, ident), writes=out.keys)

    def act(self, out, in_, func, bias=0.0, scale=1.0, accum=None, eng='act'):
        kw = {}
        if accum is not None:
            kw['accum_out'] = accum.ap
        self.P.op(eng, lambda e: e.activation(out.ap, in_.ap, func, bias=_a(bias), scale=_a(scale), **kw),
                  reads=_keys(in_, bias, scale), writes=_keys(out, accum))

    def tt(self, out, a, b, op, eng='dve'):
        self.P.op(eng, lambda e: e.tensor_tensor(out.ap, a.ap, b.ap, op), reads=_keys(a, b), writes=out.keys)

    def ts(self, out, a, s1, s2, op0, op1=None, eng='dve'):
        if op1 is None:
            self.P.op(eng, lambda e: e.tensor_scalar(out.ap, a.ap, _a(s1), None, op0),
                      reads=_keys(a, s1), writes=out.keys)
        else:
            self.P.op(eng, lambda e: e.tensor_scalar(out.ap, a.ap, _a(s1), _a(s2), op0, op1),
                      reads=_keys(a, s1, s2), writes=out.keys)

    def stt(self, out, in0, scalar, in1, op0, op1, eng='dve'):
        self.P.op(eng, lambda e: e.scalar_tensor_tensor(out.ap, in0.ap, _a(scalar), in1.ap, op0, op1),
                  reads=_keys(in0, scalar, in1), writes=out.keys)

    def cp(self, out, in_, eng='dve'):
        if eng == 'act':
            self.P.op('act', lambda e: e.copy(out.ap, in_.ap), reads=in_.keys, writes=out.keys)
        else:
            self.P.op(eng, lambda e: e.tensor_copy(out.ap, in_.ap), reads=in_.keys, writes=out.keys)

    def red(self, out, in_, op, eng='dve'):
        self.P.op(eng, lambda e: e.tensor_reduce(out.ap, in_.ap, AX.X, op), reads=in_.keys, writes=out.keys)

    def recip(self, out, in_):
        self.P.op('dve', lambda e: e.reciprocal(out.ap, in_.ap), reads=in_.keys, writes=out.keys)

    def memset(self, out, val, eng='dve'):
        self.P.op(eng, lambda e: e.memset(out.ap, val), writes=out.keys)

    def scan(self, out, d0, d1, init, op0, op1):
        self.P.op('dve', lambda e: e.tensor_tensor_scan(out.ap, d0.ap, d1.ap, _a(init), op0, op1),
                  reads=_keys(d0, d1, init), writes=out.keys)

    def dma(self, eng, chan, out, in_):
        self.P.dma(eng, chan, lambda e: e.dma_start(out=out.ap, in_=in_.ap), reads=in_.keys, writes=out.keys)

    def sb(self, name, shape, dt, key=None):
        t = self.st.enter_context(self.nc.sbuf_tensor("sb_" + name, shape, dt))
        return V(t[:], (key or name,))

    def bank(self):
        pool = self.BANKS[self.cur_pool]
        i = self.bank_pos[self.cur_pool]
        self.bank_pos[self.cur_pool] = (i + 1) % len(pool)
        return self.banks[pool[i]]

    def dram_in(self, name, shape):
        ap = self.nc.dram_tensor(name, list(shape), F32, kind="ExternalInput").ap()
        self.din[name] = ap
        return ap

    def convert(self, name, src, nk, C):
        if name in self.pids:
            return
        n = nk * C
        pid = len(self.pids)
        self.pids[name] = pid
        if pid // 256 >= len(self.scrs):
            self.scrs.append(self.nc.dram_tensor("wscr%d" % len(self.scrs), [256, 128, SLOT], BF16, kind="Internal").ap())
        ch = pid % NCVT
        self.dma('pool', ('cvt', ch),
                 V(self.scrs[pid // 256][pid % 256, :, 0:n].rearrange("p (k c) -> p k c", k=nk), (('scr', pid), ('cvtch', ch))),
                 V(src.rearrange("(k p) c -> p k c", p=128), ()))

    def wpanel(self, name, src, nk, C):
        n = nk * C
        assert n <= SLOT
        pool = self.SLOTS[self.cur_pool]
        i = self.slot_pos[self.cur_pool]
        self.slot_pos[self.cur_pool] = (i + 1) % len(pool)
        s = pool[i]
        slot = V(self.wbf[s].ap[:, 0:n], (('wbf', s),))
        if name not in self.pids:
            self.convert(name, src, nk, C)
        pid = self.pids[name]
        self.dma('sp', ('wl16', s), slot, V(self.scrs[pid // 256][pid % 256, :, 0:n], (('scr', pid),)))
        return slot.re("p (k c) -> p k c", k=nk)

    def build(self):
        nc, P, S, depth = self.nc, self.P, self.S, self.depth
        n_moe = max(1, len(self.moe_layers))
        n_dense = max(1, depth - len(self.moe_layers))
        di = self.dram_in
        x_d = di("x", (S, D))
        mem_d = di("mem", (MEM, D))
        pp_d = di("pp", (128, PP_L * depth))
        bc_d = di("bc", (128, 1032 + 8 * depth))
        w_in = di("w_in", (depth, D, N_IN))
        rg_w_a = di("rg_w_a", (depth, 4, 256, 256))
        rg_w_x = di("rg_w_x", (depth, 4, 256, 256))
        ml_w_q = di("ml_w_q", (depth, 4, 256, 256))
        ml_w_k = di("ml_w_k", (depth, 4, 256, 256))
        ml_w_v = di("ml_w_v", (depth, 4, 256, 256))
        w_kv = di("w_kv", (depth, D, 2 * D))
        w_br = [di("w_br_rg", (depth, D, D)), di("w_br_ml", (depth, D, D)), di("w_br_xa", (depth, D, D))]
        w_out = di("w_out", (depth, D, D))
        ffn_w1 = di("ffn_w1", (n_dense, D, DFF))
        ffn_w3 = di("ffn_w3", (n_dense, D, DFF))
        ffn_w2 = di("ffn_w2", (n_dense, DFF, D))
        router_w = di("router_w", (n_moe, D, NEXP))
        moe_w1 = di("moe_w1", (n_moe, NEXP, D, DFF))
        moe_w3 = di("moe_w3", (n_moe, NEXP, D, DFF))
        moe_w2 = di("moe_w2", (n_moe, NEXP, DFF, D))
        y_d = nc.dram_tensor("y", [S, D], F32, kind="ExternalOutput").ap()
        npan = depth * 110 + (len(self.moe_layers) * 8 + n_dense) * 44 + 16
        self.scrs = []

        sb = self.sb
        self.banks = []
        for b in range(8):
            t = self.st.enter_context(nc.psum_tensor("ps%d" % b, [128, 512], F32))
            self.banks.append(V(t[:], (('ps', b),)))
        self.wbf = [sb("wbf%d" % i, [128, SLOT], BF16, ('wbf', i)) for i in range(NBF)]
        xres_l = [sb("xres%d" % p_, [128, NCH, D], F32) for p_ in range(2)]
        xr_l = [[V(xres_l[p_].ap[:, c, :], (('x', p_, c),)) for c in range(NCH)] for p_ in range(2)]
        cx = {'p': 0}
        hT_t = sb("hTm", [128, KC, T], BF16)
        hTf_t = sb("hTf", [128, KC, T], BF16)
        ybr_t = sb("ybr", [128, KC, T], BF16)
        ybr = [V(ybr_t.ap[:, k, :], (('ybr', k),)) for k in range(KC)]
        mgb_t = sb("mgb", [128, KC, T], BF16)
        mgb = [V(mgb_t.ap[:, k, :], (('mgb', k),)) for k in range(KC)]
        hid_t = sb("hid", [128, NF // 2, T], BF16)
        hid = [V(hid_t.ap[:, f, :], (('hid', f),)) for f in range(NF // 2)]
        WA = sb("WA", [128, 2, T + 4], F32)
        WB = sb("WB", [128, 2, T + 4], F32)
        WC = sb("WC", [128, 2, T + 4], F32)
        WD = sb("WD", [128, 2, T + 4], F32)
        WE = sb("WE", [128, 2, T + 4], F32)
        WFb = sb("WF", [128, 2, T + 4], F32)
        h32 = V(WA.ap.rearrange("p a b -> p (a b)")[:, 0:KC * 128].rearrange("p (k t) -> p k t", k=KC), WA.keys)
        xn = V(WB.ap.rearrange("p a b -> p (a b)")[:, 0:D], WB.keys)
        B0 = sb("B0", [128, 2, T], BF16)
        B1 = sb("B1", [128, 2, T], BF16)
        B2 = sb("B2", [128, 2, T], BF16)
        B3 = sb("B3", [128, 2, T], BF16)
        B4 = sb("B4", [128, NCH, 256], BF16)
        B5 = sb("B5", [128, NCH, 260], BF16)
        Cst = sb("Cst", [128, depth * 8, 257], F32)
        Cbf = sb("Cbf", [128, depth * 8, 257], BF16)
        Kf = sb("Kf", [128, depth * KC, MEM], BF16)
        Vt = sb("Vt", [128, depth * 2, D], BF16)
        memx = V(hid_t.ap.bitcast(F32).rearrange("p a b -> p (a b)")[:, 0:2 * D].rearrange("p (c d) -> p c d", c=2),
                 tuple(k for h_ in hid for k in h_.keys))
        pp = sb("pp", [128, PP_L * depth], F32)
        bcp = sb("bcp", [128, 1032 + 8 * depth], F32)
        id32 = sb("id32", [128, 128], F32)
        idbf = sb("idbf", [128, 128], BF16)
        tri = sb("tri", [128, 128], F32)
        ones = sb("ones", [128, 128], F32)
        maskT = sb("maskT", [128, 128], F32)
        halo = sb("halo", [128, depth * 2 * KC, 4], F32)
        hst = sb("hst", [128, depth * KC], F32)
        cA = sb("cA", [128, depth * KC], F32)
        cA2 = sb("cA2", [128, depth * KC], F32)
        sm = sb("sm", [128, 64], F32)
        gsc = sb("gsc", [128, 6, 16], F32)
        rw32 = sb("rw32", [128, KC, NEXP], F32)
        rgate = sb("rgate", [128, NCH, NEXP], F32)
        rtmp = sb("rtmp", [128, 4, NEXP], F32)
        ST = sb("ST", [128, 128], BF16)
        Vs = sb("Vs", [128, 260], BF16)
        VsL = sb("VsL", [128, 260], BF16)
        hm = sb("hm", [128, 256], F32)
        hnb = sb("hnb", [128, 256], BF16)
        ex = sb("ex", [128, 256], F32)
        pb = sb("pb", [128, 256], BF16)
        silt = sb("silt", [128, T], F32)

        self.dma('pool', 'c0', pp, V(pp_d, ()))
        self.dma('pool', 'c1', bcp, V(bc_d, ()))
        P.op('pool', lambda e: e.memset(ones.ap, 1.0), writes=ones.keys)
        P.op('pool', lambda e: e.affine_select(id32.ap, ones.ap, [[-1, 128]], ALU.is_equal, 0.0, base=0, channel_multiplier=1),
             reads=ones.keys, writes=id32.keys)
        P.op('pool', lambda e: e.affine_select(tri.ap, ones.ap, [[1, 128]], ALU.is_ge, 0.0, base=0, channel_multiplier=-1),
             reads=ones.keys, writes=tri.keys)
        self.cp(idbf, id32, eng='pool')
        self.cp(maskT, tri, eng='pool')
        self.memset(halo, 0.0, eng='pool')
        self.memset(hst, 0.0, eng='pool')
        self.memset(Cst, 0.0, eng='pool')
        self.memset(Cbf, 0.0, eng='pool')
        for l in range(depth):
            lam = pp[:, l * PP_L + PP_OFF["lam"]: l * PP_L + PP_OFF["lam"] + 8]
            c = cA[:, l * 8:(l + 1) * 8]
            c2 = cA2[:, l * 8:(l + 1) * 8]
            self.act(c, lam, AF.Exp, scale=-1.0)
            self.act(c, c, AF.Ln, bias=1.0)
            self.ts(c2, c, -16.0, None, ALU.mult)
            self.ts(c, c, -8.0, None, ALU.mult)
        for j, l in enumerate(self.moe_layers):
            assert j == 0
            self.dma('pool', 'c2', rw32, V(router_w[j].rearrange("(k p) e -> p k e", p=128), ()))

        def ppc(l, name, c, n=1):
            o = l * PP_L + PP_OFF[name] + c
            return pp[:, o:o + n]

        def norm_T(src_tc, gname, l, tc_idx, dst_hT, ncols, want32=False):
            ss = sm[:, 0:1]
            rs = sm[:, 1:2]
            self.act(xn, src_tc, AF.Square, accum=ss)
            self.act(rs, ss, AF.Sqrt, bias=EPS, scale=1.0 / D)
            self.recip(rs, rs)
            self.ts(xn, src_tc, rs, None, ALU.mult)
            for half in range(2):
                bk = self.bank()
                for q in range(4):
                    k = half * 4 + q
                    self.tr(bk[:, q * 128:(q + 1) * 128], xn[:, k * 128:(k + 1) * 128], id32)
                g = ppc(l, gname, half * 4, 4)
                gb = V(g.ap.unsqueeze(2).to_broadcast([128, 4, 128]), g.keys)
                src = bk.re("p (q t) -> p q t", q=4)
                self.tt(dst_hT[:, half * 4:half * 4 + 4, tc_idx * 128:(tc_idx + 1) * 128], src, gb, ALU.mult)
                if want32:
                    self.tt(h32[:, half * 4:half * 4 + 4, :], src, gb, ALU.mult, eng='dve')

        def mem_kv(l):
            self.dma('pool', 'memld', memx, V(mem_d.rearrange("(c p) d -> p c d", p=128), ()))
            memT = ybr_t
            for c in range(2):
                norm_T(memx[:, c, :], "gmem", l, c, V(memT.ap, tuple(k for y in ybr for k in y.keys)), 256)
            mT = [V(ybr_t.ap[:, k, 0:MEM], ybr[k].keys) for k in range(KC)]
            for pi in range(4):
                wp = self.wpanel(("wkvK", l, pi), w_kv[l][:, pi * 256:(pi + 1) * 256], KC, 256)
                for m2 in range(2):
                    mo = pi * 2 + m2
                    bk = self.bank()
                    for k in range(KC):
                        self.mm(bk[:, 0:MEM], wp[:, k, m2 * 128:(m2 + 1) * 128], mT[k], start=(k == 0), stop=(k == KC - 1))
                    self.cp(Kf[:, l * KC + mo, :], bk[:, 0:MEM], eng='act')
            for pi in range(4):
                wp = self.wpanel(("wkvV", l, pi), w_kv[l][:, D + pi * 256:D + (pi + 1) * 256], KC, 256)
                for mc in range(2):
                    bk = self.bank()
                    for k in range(KC):
                        self.mm(bk[:, 0:256], mT[k][:, mc * 128:(mc + 1) * 128], wp[:, k, :], start=(k == 0), stop=(k == KC - 1))
                    self.cp(Vt[:, l * 2 + mc, pi * 256:(pi + 1) * 256], bk[:, 0:256], eng='act')

        hT = [V(hT_t.ap[:, k, :], hT_t.keys) for k in range(KC)]

        def proj_fm(wp, m2, rhs_list, nk, bk, N=T, tk=False):
            for k in range(nk):
                self.mm(bk[:, 0:N], wp[:, k, m2 * 128:(m2 + 1) * 128], rhs_list[k], start=(k == 0), stop=(k == nk - 1))
            if tk:
                self.tick()

        def conv_group(l, which, g, src_banks, dst, hidx0):
            wn, bn = ("crw", "crb") if which == 0 else ("cmw", "cmb")
            for m in range(2):
                ch = g * 2 + m
                hx = halo[:, (l * 2 + which) * KC + ch, 0:3]
                self.cp(WA[:, m, 0:3], hx, eng='dve')
                self.cp(WA[:, m, 3:3 + T], src_banks[m], eng='act')
                self.cp(hx, WA[:, m, T:T + 3], eng='dve')
                o = dst[:, m, 0:T]
                self.ts(o, WA[:, m, 0:T], ppc(l, wn, 0 * 8 + ch), ppc(l, bn, ch), ALU.mult, ALU.add)
                for j in range(1, 4):
                    self.stt(o, WA[:, m, j:j + T], ppc(l, wn, j * 8 + ch), o, ALU.mult, ALU.add)

        def merge_branch(l, b, ysrc):
            for pi in range(4):
                wpb = self.wpanel(("wbr", l, b, pi), w_br[b][l][:, pi * 256:(pi + 1) * 256], KC, 256)
                wpg = self.wpanel(("wg", l, b, pi), w_in[l][:, 5128 + b * D + pi * 256: 5128 + b * D + (pi + 1) * 256], KC, 256)
                for m2 in range(2):
                    mo = pi * 2 + m2
                    bg = self.bank()
                    proj_fm(wpg, m2, hT, KC, bg)
                    bp = self.bank()
                    proj_fm(wpb, m2, ysrc, KC, bp)
                    gt = WB[:, 0, 0:T]
                    self.act(gt, bg, AF.Sigmoid, bias=ppc(l, "bmg", b * 8 + mo))
                    self.tt(mgb[mo], bp, gt, ALU.mult)
            for pi in range(4):
                wpo = self.wpanel(("wout", l, pi), w_out[l][:, pi * 256:(pi + 1) * 256], KC, 256)
                for tc in range(NCH):
                    bk = self.bank()
                    for k in range(KC):
                        self.mm(bk[:, 0:256], mgb[k][:, tc * 128:(tc + 1) * 128], wpo[:, k, :], start=(k == 0), stop=(k == KC - 1))
                    xs = xr_l[cx['p']][tc][:, pi * 256:(pi + 1) * 256]
                    self.tt(xs, bk[:, 0:256], xs, ALU.add)

        def rg_branch(l):
            for g in range(4):
                wp = self.wpanel(("ax", l, g), w_in[l][:, g * 256:(g + 1) * 256], KC, 256)
                bks = []
                for m in range(2):
                    bk = self.bank()
                    proj_fm(wp, m, hT, KC, bk)
                    bks.append(bk)
                conv_group(l, 0, g, bks, WB, None)
                xcb = B0
                self.cp(xcb[:, :, :], WB[:, :, 0:T], eng='dve')
                wa = self.wpanel(("rga", l, g), rg_w_a[l, g], 2, 256)
                wx = self.wpanel(("rgx", l, g), rg_w_x[l, g], 2, 256)
                xcl = [xcb[:, 0, :], xcb[:, 1, :]]
                for m in range(2):
                    ch = g * 2 + m
                    br_ = self.bank()
                    proj_fm(wa, m, xcl, 2, br_)
                    bi_ = self.bank()
                    proj_fm(wx, m, xcl, 2, bi_)
                    r = WC[:, m, 0:T]
                    self.act(r, br_, AF.Sigmoid, bias=ppc(l, "rba", ch))
                    iv = WFb[:, m, 0:T]
                    self.act(iv, bi_, AF.Sigmoid, bias=ppc(l, "rbx", ch))
                    a = WD[:, m, 0:T]
                    self.act(a, r, AF.Exp, scale=cA[:, l * 8 + ch:l * 8 + ch + 1])
                    s = WE[:, m, 0:T]
                    self.act(s, r, AF.Exp, scale=cA2[:, l * 8 + ch:l * 8 + ch + 1])
                    self.act(s, s, AF.Sqrt, bias=1.0, scale=-1.0)
                    self.tt(s, s, iv, ALU.mult)
                    self.tt(s, s, WB[:, m, 0:T], ALU.mult)
                    hs = WC[:, m, 0:T]
                    hcol = hst[:, l * 8 + ch:l * 8 + ch + 1]
                    self.scan(hs, a, s, hcol, ALU.mult, ALU.add)
                    self.cp(hcol, hs[:, T - 1:T], eng='pool')
                wy = self.wpanel(("ay", l, g), w_in[l][:, D + g * 256: D + (g + 1) * 256], KC, 256)
                for m in range(2):
                    ch = g * 2 + m
                    bk = self.bank()
                    proj_fm(wy, m, hT, KC, bk)
                    ay = WD[:, m, 0:T]
                    self.cp(ay, bk, eng='act')
                    u = WE[:, m, 0:T]
                    self.act(u, ay, AF.Square)
                    self.ts(u, u, 0.044715, 1.0, ALU.mult, ALU.add)
                    self.tt(u, u, ay, ALU.mult)
                    self.act(u, u, AF.Sigmoid, scale=1.5957691216057308)
                    self.tt(u, u, ay, ALU.mult, eng='pool')
                    self.tt(ybr[ch], u, WC[:, m, 0:T], ALU.mult, eng='pool')
            self.marks.append((-1, l, 'rg_merge', dict(P.count)))
            merge_branch(l, 0, ybr)

        def ml_gates(l):
            wp = self.wpanel(("wif", l), w_in[l][:, 4096:4104], KC, 8)
            bk = self.bank()
            for c in range(NCH):
                for k in range(KC):
                    self.mm(bk[:, c * 8:(c + 1) * 8], hT[k][:, c * 128:(c + 1) * 128], wp[:, k, :], start=(k == 0), stop=(k == KC - 1))
            pre = bk[:, 0:NCH * 8].re("p (c e) -> p c e", c=NCH)
            bif = bcp[:, BC_BI + l * 8: BC_BI + l * 8 + 8]
            ig = gsc[:, 4, :].re("p (c h) -> p c h", c=NCH)
            fg = gsc[:, 5, :].re("p (c h) -> p c h", c=NCH)
            bi_b = V(bif.ap[:, 0:4].unsqueeze(1).to_broadcast([128, NCH, 4]), bif.keys)
            bf_b = V(bif.ap[:, 4:8].unsqueeze(1).to_broadcast([128, NCH, 4]), bif.keys)
            self.tt(ig, pre[:, :, 0:4], bi_b, ALU.add)
            self.tt(fg, pre[:, :, 4:8], bf_b, ALU.add)
            sp = gsc[:, 5, :]
            self.act(sp, sp, AF.Exp, scale=-1.0)
            self.act(sp, sp, AF.Ln, bias=1.0)
            b2 = self.bank()
            self.mm(b2[:, 0:16], tri, sp)
            self.mm(b2[:, 16:32], ones, sp)
            self.tt(gsc[:, 0, :], gsc[:, 4, :], b2[:, 0:16], ALU.add)
            self.act(gsc[:, 0, :], gsc[:, 0, :], AF.Exp)
            self.act(gsc[:, 2, :], b2[:, 0:16], AF.Exp)
            self.act(gsc[:, 3, :], b2[:, 16:32], AF.Exp, scale=-1.0)
            self.tt(gsc[:, 1, :], gsc[:, 0, :], gsc[:, 3, :], ALU.mult)

        def ml_branch(l):
            ml_gates(l)
            for g in range(4):
                wp = self.wpanel(("mu", l, g), w_in[l][:, 2048 + g * 256: 2048 + (g + 1) * 256], KC, 256)
                bks = []
                for m in range(2):
                    bk = self.bank()
                    proj_fm(wp, m, hT, KC, bk)
                    bks.append(bk)
                ub = B1
                for m in range(2):
                    self.cp(ub[:, m, :], bks[m], eng='dve')
                conv_group(l, 1, g, bks, WB, None)
                cb = B0
                self.act(cb[:, :, :], WB[:, :, 0:T], AF.Silu)
                cbl = [cb[:, 0, :], cb[:, 1, :]]
                qf, kf, ktok, vaug = B2, B3, B4, B5
                self.memset(vaug, 1.0, eng='pool')
                wq = self.wpanel(("mq", l, g), ml_w_q[l, g], 2, 256)
                for m in range(2):
                    bk = self.bank()
                    proj_fm(wq, m, cbl, 2, bk)
                    self.cp(qf[:, m, :], bk, eng='act')
                wk = self.wpanel(("mk", l, g), ml_w_k[l, g], 2, 256)
                for m in range(2):
                    bk = self.bank()
                    proj_fm(wk, m, cbl, 2, bk)
                    self.act(kf[:, m, :], bk, AF.Copy, scale=1.0 / 16.0)
                for c in range(NCH):
                    cs = slice(c * 128, (c + 1) * 128)
                    bk = self.bank()
                    for m in range(2):
                        self.mm(bk[:, 0:256], cb[:, m, cs], wk[:, m, :], start=(m == 0), stop=(m == 1))
                    self.act(ktok[:, c, :], bk[:, 0:256], AF.Copy, scale=1.0 / 16.0)
                wv = self.wpanel(("mv", l, g), ml_w_v[l, g], 2, 256)
                for c in range(NCH):
                    cs = slice(c * 128, (c + 1) * 128)
                    bk = self.bank()
                    for m in range(2):
                        self.mm(bk[:, 0:256], ub[:, m, cs], wv[:, m, :], start=(m == 0), stop=(m == 1))
                    self.cp(vaug[:, c, 0:256], bk[:, 0:256], eng='dve')
                wo = self.wpanel(("mo", l, g), w_in[l][:, 3072 + g * 256: 3072 + (g + 1) * 256], KC, 256)
                otok = WD
                ot = V(WD.ap.rearrange("p a b -> p (a b)")[:, 0:NCH * 256].rearrange("p (c d) -> p c d", c=NCH), WD.keys)
                for c in range(NCH):
                    bk = self.bank()
                    for k in range(KC):
                        self.mm(bk[:, 0:256], hT[k][:, c * 128:(c + 1) * 128], wo[:, k, :], start=(k == 0), stop=(k == KC - 1))
                    self.act(ot[:, c, :], bk[:, 0:256], AF.Sigmoid)
                self.marks.append((-1, l, 'ml_chunks%d' % g, dict(P.count)))
                for c in range(NCH):
                    cs = slice(c * 128, (c + 1) * 128)
                    col = c * 4 + g
                    ec = gsc[:, 0, col:col + 1]
                    ecL = gsc[:, 1, col:col + 1]
                    enb = gsc[:, 2, col:col + 1]
                    ebL = gsc[:, 3, col:col + 1]
                    bs = self.bank()
                    for m in range(2):
                        self.mm(bs[:, 0:128], kf[:, m, cs], qf[:, m, cs], start=(m == 0), stop=(m == 1))
                    self.tt(ST, bs[:, 0:128], maskT, ALU.mult)
                    self.ts(Vs[:, 0:257], vaug[:, c, 0:257], ec, None, ALU.mult)
                    self.act(VsL[:, 0:257], vaug[:, c, 0:257], AF.Copy, scale=ecL)
                    bn = self.bank()
                    for m in range(2):
                        self.mm(bn[:, 0:257], qf[:, m, cs], Cbf[:, l * 8 + g * 2 + m, :], start=(m == 0), stop=False)
                    self.mm(bn[:, 0:257], ST, Vs[:, 0:257], start=False, stop=True)
                    den = sm[:, 8:9]
                    self.ts(den, bn[:, 256:257], -1.0, None, ALU.mult)
                    self.tt(den, den, bn[:, 256:257], ALU.max)
                    self.tt(den, den, enb, ALU.max)
                    self.recip(den, den)
                    self.stt(hm, bn[:, 0:256], den, ot[:, c, :], ALU.mult, ALU.mult)
                    ss = sm[:, 9:10]
                    self.act(ex, hm, AF.Square, accum=ss)
                    self.act(ss, ss, AF.Sqrt, bias=EPS, scale=1.0 / 256.0)
                    self.recip(ss, ss)
                    self.ts(hnb, hm, ss, None, ALU.mult)
                    bt = self.bank()
                    btb = bt.cast(BF16)
                    for m in range(2):
                        self.tr(btb[:, m * 128:(m + 1) * 128], hnb[:, m * 128:(m + 1) * 128], idbf)
                    for m in range(2):
                        self.ts(ybr[g * 2 + m][:, cs], btb[:, m * 128:(m + 1) * 128], ppc(l, "gml", g * 2 + m), None, ALU.mult)
                    for m in range(2):
                        bd = self.bank()
                        self.mm(bd[:, 0:257], ktok[:, c, m * 128:(m + 1) * 128], VsL[:, 0:257])
                        Cm = Cst[:, l * 8 + g * 2 + m, :]
                        self.stt(Cm, Cm, ebL, bd[:, 0:257], ALU.mult, ALU.add)
                        self.cp(Cbf[:, l * 8 + g * 2 + m, :], Cm, eng='act')
            self.marks.append((-1, l, 'ml_merge', dict(P.count)))
            merge_branch(l, 1, ybr)

        def xa_branch(l):
            for g in range(4):
                wp = self.wpanel(("xq", l, g), w_in[l][:, 4104 + g * 256: 4104 + (g + 1) * 256], KC, 256)
                qf = B2
                for m in range(2):
                    bk = self.bank()
                    proj_fm(wp, m, hT, KC, bk)
                    self.act(qf[:, m, :], bk, AF.Copy, scale=1.0 / 16.0)
                PT = B1
                for c in range(NCH):
                    cs = slice(c * 128, (c + 1) * 128)
                    bk = self.bank()
                    for m in range(2):
                        self.mm(bk[:, 0:MEM], qf[:, m, cs], Kf[:, l * KC + g * 2 + m, :], start=(m == 0), stop=(m == 1))
                    mx = sm[:, 12:13]
                    self.red(mx, bk[:, 0:MEM], ALU.max)
                    self.ts(mx, mx, -1.0, None, ALU.mult)
                    ssum = sm[:, 13:14]
                    self.act(ex, bk[:, 0:MEM], AF.Exp, bias=mx, accum=ssum)
                    self.recip(ssum, ssum)
                    self.ts(pb, ex, ssum, None, ALU.mult)
                    bt = self.bank()
                    btb = bt.cast(BF16)
                    for mc in range(2):
                        self.tr(btb[:, mc * 128:(mc + 1) * 128], pb[:, mc * 128:(mc + 1) * 128], idbf)
                    self.cp(PT[:, :, cs], btb[:, 0:256].re("p (a t) -> p a t", a=2), eng='act')
                for m2 in range(2):
                    bk = self.bank()
                    for mc in range(2):
                        self.mm(bk, Vt[:, l * 2 + mc, g * 256 + m2 * 128: g * 256 + (m2 + 1) * 128], PT[:, mc, :],
                                start=(mc == 0), stop=(mc == 1))
                    self.cp(ybr[g * 2 + m2], bk, eng='act')
            self.marks.append((-1, l, 'xa_merge', dict(P.count)))
            merge_branch(l, 2, ybr)

        def ffn(w1, w3, w2, name, hsrc, gate_e=None):
            nf_h = NF // 2
            for half in range(2):
                f0 = half * nf_h
                for pj in range(nf_h // 2):
                    c0 = (f0 + pj * 2) * 128
                    pa = self.wpanel((name, "w1", half, pj), w1[:, c0:c0 + 256], KC, 256)
                    pb3 = self.wpanel((name, "w3", half, pj), w3[:, c0:c0 + 256], KC, 256)
                    for m2 in range(2):
                        up_chunk(pa, pb3, m2, hid[pj * 2 + m2], hsrc)
                for ch in range(2):
                    accs = [self.bank() for _ in range(NCH)]
                    for fp in range(0, nf_h, 4):
                        nfc = min(4, nf_h - fp)
                        r0 = (f0 + fp) * 128
                        wp = self.wpanel((name, "w2", half, ch, fp), w2[r0:r0 + nfc * 128, ch * 512:(ch + 1) * 512], nfc, 512)
                        for tc in range(NCH):
                            for f in range(nfc):
                                fi = fp + f
                                self.mm(accs[tc], hid[fi][:, tc * 128:(tc + 1) * 128], wp[:, f, :],
                                        start=(fi == 0), stop=(fi == nf_h - 1))
                                if f % 2 == 1:
                                    self.tick()
                    for tc in range(NCH):
                        xs = xr_l[cx['p']][tc][:, ch * 512:(ch + 1) * 512]
                        if gate_e is None:
                            self.tt(xs, accs[tc], xs, ALU.add)
                        else:
                            self.stt(xs, accs[tc], rgate[:, tc, gate_e:gate_e + 1], xs, ALU.mult, ALU.add)

        def ffn_convert(w1, w3, w2, name):
            nf_h = NF // 2
            for half in range(2):
                f0 = half * nf_h
                for pj in range(nf_h // 2):
                    c0 = (f0 + pj * 2) * 128
                    self.convert((name, "w1", half, pj), w1[:, c0:c0 + 256], KC, 256)
                    self.convert((name, "w3", half, pj), w3[:, c0:c0 + 256], KC, 256)
                for ch in range(2):
                    for fp in range(0, nf_h, 4):
                        nfc = min(4, nf_h - fp)
                        r0 = (f0 + fp) * 128
                        self.convert((name, "w2", half, ch, fp), w2[r0:r0 + nfc * 128, ch * 512:(ch + 1) * 512], nfc, 512)

        def up_chunk(pa, pb3, m2, hdst, hsrc):
            b1 = self.bank()
            proj_fm(pa, m2, hsrc, KC, b1, tk=True)
            b3 = self.bank()
            proj_fm(pb3, m2, hsrc, KC, b3, tk=True)
            s = silt
            self.act(s, b1, AF.Silu)
            self.tt(hdst, b3, s, ALU.mult)

        def router(l, tc):
            bk = self.bank()
            for k in range(KC):
                self.mm(bk[:, 0:NEXP], h32[:, k, :], rw32[:, k, :], start=(k == 0), stop=(k == KC - 1))
            lg = rtmp[:, 0, :]
            self.tt(lg, bk[:, 0:NEXP], bcp[:, BC_RB:BC_RB + NEXP], ALU.add)
            m1 = sm[:, 16:17]
            self.red(m1, lg, ALU.max)
            eq = rtmp[:, 1, :]
            self.ts(eq, lg, m1, None, ALU.is_equal)
            l2 = rtmp[:, 2, :]
            self.stt(l2, eq, -1e30, lg, ALU.mult, ALU.add)
            m2 = sm[:, 17:18]
            self.red(m2, l2, ALU.max)
            sel = rtmp[:, 1, :]
            self.ts(sel, lg, m2, None, ALU.is_ge)
            nm1 = sm[:, 18:19]
            self.ts(nm1, m1, -1.0, None, ALU.mult)
            e = rtmp[:, 3, :]
            self.act(e, lg, AF.Exp, bias=nm1)
            self.tt(e, e, sel, ALU.mult)
            sume = sm[:, 19:20]
            self.red(sume, e, ALU.add)
            self.recip(sume, sume)
            self.ts(rgate[:, tc, :], e, sume, None, ALU.mult)

        hT_all = V(hT_t.ap, hT_t.keys)
        hTf = [V(hTf_t.ap[:, k, :], hTf_t.keys) for k in range(KC)]
        hTf_all = V(hTf_t.ap, hTf_t.keys)
        NT = self.NT
        assert depth == 2 and self.moe_layers == [1]

        def allx(p_):
            return V(xres_l[p_].ap, xres_l[p_].keys + tuple(k for c in range(NCH) for k in xr_l[p_][c].keys))

        def xtc(p_, tc):
            return V(xres_l[p_].ap[:, tc, :], xr_l[p_][tc].keys + xres_l[p_].keys)

        def load_x(t):
            p_ = t % 2
            self.dma('pool', 'xld', allx(p_), V(x_d[t * T:(t + 1) * T, :].rearrange("(c p) d -> p c d", p=128), ()))

        def mixer(t, l):
            cx['p'] = t % 2
            self.cur_pool = 'm'
            if t == 0:
                mem_kv(l)
            for tc in range(NCH):
                norm_T(xtc(t % 2, tc), "g_mix", l, tc, hT_all, T)
            rg_branch(l)
            ml_branch(l)
            xa_branch(l)

        def dense_ffn(t, l):
            cx['p'] = t % 2
            self.cur_pool = 'f'
            for tc in range(NCH):
                norm_T(xtc(t % 2, tc), "g_ffn", l, tc, hT_all, T)
            j = l // 2
            ffn(ffn_w1[j], ffn_w3[j], ffn_w2[j], ("ffn", l), hT)

        def moe_pre(t, l):
            cx['p'] = t % 2
            self.cur_pool = 'f'
            for tc in range(NCH):
                norm_T(xtc(t % 2, tc), "g_ffn", l, tc, hTf_all, T, want32=True)
                router(l, tc)

        def final_store(t):
            p_ = t % 2
            self.cur_pool = 'f'
            for tc in range(NCH):
                xs = xtc(p_, tc)
                ss = sm[:, 0:1]
                rs = sm[:, 1:2]
                self.act(xn, xs, AF.Square, accum=ss)
                self.act(rs, ss, AF.Sqrt, bias=EPS, scale=1.0 / D)
                self.recip(rs, rs)
                self.stt(xs, xs, rs, bcp[:, BC_FINAL:BC_FINAL + D], ALU.mult, ALU.mult)
            self.dma('pool', 'yst', V(y_d[t * T:(t + 1) * T, :].rearrange("(c p) d -> p c d", p=128), ('y',)), allx(p_))

        def record(fn):
            P.begin_defer()
            fn()
            return P.end_defer()

        def experts(t, l, es, seg, delay=0):
            cx['p'] = t % 2
            self.cur_pool = 'f'
            n_units = len(es) * 2 * 84
            if seg and 'nodefer' in dbg:
                P.feed(seg, 0, len(seg))
                seg = None
            self.feed_lst = seg
            self.feed_pos = 0
            self.feed_delay = delay
            self.ticks_left = n_units - 8
            if seg and delay > 0:
                self.feed_pos = P.feed(seg, 0, 1)
            j = self.moe_layers.index(l)
            for e_ in es:
                ffn(moe_w1[j, e_], moe_w3[j, e_], moe_w2[j, e_], ("moe", l, e_), hTf, gate_e=e_)
            if seg:
                self.leftover.append(len(seg) - self.feed_pos)
                self.feed_pos = P.feed(seg, self.feed_pos, len(seg))
            self.feed_lst = None

        import os
        dbg = os.environ.get("KDBG", "")

        def startup():
            load_x(0)
            mixer(0, 0)
            dense_ffn(0, 0)
            mixer(0, 1)
            moe_pre(0, 1)

        def all_moe_convert():
            for e_ in range(NEXP):
                ffn_convert(moe_w1[0, e_], moe_w3[0, e_], moe_w2[0, e_], ("moe", 1, e_))

        if 'noprecvt' in dbg:
            startup()
        else:
            segS = record(startup)
            segC = record(all_moe_convert)
            LEAD = 12
            is_cvt = [(r[0] == 1 and isinstance(r[2], tuple) and r[2][0] == 'cvt') for r in segS]
            cvt_idx = [i for i, c in enumerate(is_cvt) if c]
            fed_c = 0
            seen_c = 0
            pc_ = 0
            ratio = max(1, (len(segS) - len(cvt_idx)) // max(1, len(segC)))
            cnt_ = 0
            for i, r in enumerate(segS):
                if is_cvt[i]:
                    seen_c += 1
                    continue
                while fed_c < min(len(cvt_idx), seen_c + LEAD):
                    P.feed(segS, cvt_idx[fed_c], 1)
                    fed_c += 1
                P.feed(segS, i, 1)
                cnt_ += 1
                if cnt_ % ratio == 0:
                    pc_ = P.feed(segC, pc_, 1)
            while fed_c < len(cvt_idx):
                P.feed(segS, cvt_idx[fed_c], 1)
                fed_c += 1
            pc_ = P.feed(segC, pc_, len(segC))
        for t in range(NT):
            if t + 1 < NT:
                segA = record(lambda: (load_x(t + 1), mixer(t + 1, 0)))
                experts(t, 1, [0, 1, 2, 3], segA, delay=48)
                dense_ffn(t + 1, 0)
                segB = record(lambda: mixer(t + 1, 1))
                experts(t, 1, [4, 5, 6, 7], segB)
            else:
                experts(t, 1, list(range(NEXP)), None)
            final_store(t)
            if t + 1 < NT:
                moe_pre(t + 1, 1)
        P.wait_all('pool', ['y'])
        P.emit()
        self.st.close()
        return nc


def pack_params(inp, depth):
    def col(a):
        return np.ascontiguousarray(np.asarray(a, np.float32).reshape(-1, 128).T)
    pp = np.zeros((128, PP_L * depth), np.float32)
    for l in range(depth):
        o = l * PP_L
        def put(name, arr):
            a = PP_OFF[name]
            pp[:, o + a:o + a + arr.shape[1]] = arr
        put("g_mix", col(inp["norm_mix_g"][l]))
        put("g_ffn", col(inp["norm_ffn_g"][l]))
        put("crw", np.concatenate([col(inp["conv_rg_w"][l][j]) for j in range(4)], axis=1))
        put("crb", col(inp["conv_rg_b"][l]))
        put("rba", col(inp["rg_b_a"][l]))
        put("rbx", col(inp["rg_b_x"][l]))
        put("lam", col(inp["rg_lambda"][l]))
        put("cmw", np.concatenate([col(inp["conv_ml_w"][l][j]) for j in range(4)], axis=1))
        put("cmb", col(inp["conv_ml_b"][l]))
        put("gml", col(inp["ml_norm_g"][l]))
        put("gmem", col(inp["mem_norm_g"][l]))
        put("bmg", col(inp["b_merge"][l]))
    bc = np.zeros((1032 + 8 * depth,), np.float32)
    bc[0:1024] = np.asarray(inp["final_norm_g"], np.float32)
    bc[1024:1032] = np.asarray(inp["router_b"], np.float32).reshape(-1)[:8]
    for l in range(depth):
        bc[1032 + l * 8:1032 + l * 8 + 4] = np.asarray(inp["ml_b_i"][l], np.float32)
        bc[1032 + l * 8 + 4:1032 + l * 8 + 8] = np.asarray(inp["ml_b_f"][l], np.float32)
    bcr = np.ascontiguousarray(np.broadcast_to(bc[None, :], (128, bc.shape[0])))
    return pp, bcr


W_NAMES = ["w_in", "rg_w_a", "rg_w_x", "ml_w_q", "ml_w_k", "ml_w_v", "w_kv", "w_br_rg", "w_br_ml", "w_br_xa",
           "w_out", "ffn_w1", "ffn_w3", "ffn_w2", "router_w", "moe_w1", "moe_w3", "moe_w2"]

_CACHE = {}


def run(inp, S, depth, moe_layers, n_cores, stages="kv,norm,rg,ml,xa,ffn"):
    key = (S, depth, tuple(moe_layers), stages)
    if key not in _CACHE:
        _CACHE[key] = K(S, depth, list(moe_layers), stages).build()
    nc = _CACHE[key]
    pp, bcr = pack_params(inp, depth)
    n_moe = max(1, len(moe_layers))
    n_dense = max(1, depth - len(moe_layers))

    def lead(n):
        return n_dense if n.startswith("ffn_") else (n_moe if (n.startswith("moe_") or n == "router_w") else depth)
    shared = {n: np.ascontiguousarray(np.asarray(inp[n], np.float32)[:lead(n)]) for n in W_NAMES}
    shared["pp"] = pp
    shared["bc"] = bcr
    in_maps = []
    for c in range(n_cores):
        m = dict(shared)
        m["x"] = np.ascontiguousarray(np.asarray(inp["x"][c], np.float32))
        m["mem"] = np.ascontiguousarray(np.asarray(inp["mem"][c], np.float32))
        in_maps.append(m)
    res = run_bass_kernel_spmd(nc, in_maps, core_ids=list(range(n_cores)))
    return np.stack([np.asarray(r["y"]) for r in res.results], axis=0)


def kernel(**inputs):
    return run(inputs, 4096, 2, (1,), 8).astype(np.float32)
```

```python
import numpy as np
from contextlib import ExitStack
import concourse.bass as bass
import concourse.mybir as mybir
from concourse.bass_utils import run_bass_kernel_spmd

F32 = mybir.dt.float32
BF16 = mybir.dt.bfloat16
AF = mybir.ActivationFunctionType
ALU = mybir.AluOpType
AX = mybir.AxisListType

D = 1024
KC = 8
T = 512
NCH = 4
MEM = 256
DFF = 3584
NF = 28
NEXP = 8
N_IN = 8200
EPS = 1e-6
SLOT = 2048
NBF = 8
NCVT = 8
NSTG = 3

PP_NAMES = [("g_mix", 8), ("g_ffn", 8), ("crw", 32), ("crb", 8), ("rba", 8), ("rbx", 8), ("lam", 8),
            ("cmw", 32), ("cmb", 8), ("gml", 8), ("gmem", 8), ("bmg", 24)]
PP_OFF = {}
_o = 0
for _n, _c in PP_NAMES:
    PP_OFF[_n] = _o
    _o += _c
PP_L = _o
BC_FINAL = 0
BC_RB = 1024
BC_BI = 1032


class V:
    __slots__ = ("ap", "keys")

    def __init__(self, ap, keys):
        self.ap = ap
        self.keys = tuple(keys)

    def __getitem__(self, idx):
        return V(self.ap[idx], self.keys)

    def re(self, pat, **kw):
        return V(self.ap.rearrange(pat, **kw), self.keys)

    def bc(self, shape):
        return V(self.ap.to_broadcast(shape), self.keys)

    def cast(self, dt):
        return V(self.ap.bitcast(dt), self.keys)


class Prog:
    ENG = ('pe', 'act', 'dve', 'pool', 'sp')

    def __init__(self, nc):
        self.nc = nc
        self.streams = {e: [] for e in self.ENG}
        self.count = {}
        self.last_w = {}
        self.readers = {}
        self.waited = {e: {} for e in self.ENG}
        self.semnames = list(self.ENG)
        self.deferred = None

    def _deps(self, eng, reads, writes):
        deps = []
        for k in reads:
            p = self.last_w.get(k)
            if p is not None:
                deps.append(p)
        for k in writes:
            p = self.last_w.get(k)
            if p is not None:
                deps.append(p)
            r = self.readers.get(k)
            if r:
                deps.extend(r.values())
        need = {}
        wd = self.waited[eng]
        for (s, v) in deps:
            if s == 'pe' and eng == 'pe':
                continue
            if wd.get(s, 0) >= v:
                continue
            if need.get(s, 0) < v:
                need[s] = v
        for s, v in need.items():
            wd[s] = v
            self.streams[eng].append(('wait', s, v))

    def _commit(self, prod, reads, writes):
        for k in reads:
            self.readers.setdefault(k, {})[prod[0]] = prod
        for k in writes:
            self.last_w[k] = prod
            self.readers[k] = {}

    def op(self, eng, fn, reads=(), writes=()):
        if self.deferred is not None:
            self.deferred.append((0, eng, fn, tuple(reads), tuple(writes)))
            return
        self._op(eng, fn, reads, writes)

    def dma(self, eng, chan, fn, reads=(), writes=()):
        if self.deferred is not None:
            self.deferred.append((1, eng, chan, fn, tuple(reads), tuple(writes)))
            return
        self._dma(eng, chan, fn, reads, writes)

    def begin_defer(self):
        self.deferred = []

    def end_defer(self):
        lst = self.deferred
        self.deferred = None
        return lst

    def feed(self, lst, pos, n):
        assert self.deferred is None
        end = min(len(lst), pos + n)
        for i in range(pos, end):
            r = lst[i]
            if r[0] == 0:
                self._op(r[1], r[2], r[3], r[4])
            else:
                self._dma(r[1], r[2], r[3], r[4], r[5])
        return end

    def _op(self, eng, fn, reads=(), writes=()):
        if eng != 'pe':
            ex = tuple(k for k in reads if isinstance(k, tuple) and k and k[0] == 'ps')
            if ex:
                writes = tuple(writes) + ex
        self._deps(eng, reads, writes)
        v = self.count.get(eng, 0) + 1
        self.count[eng] = v
        self.streams[eng].append(('op', fn, eng, 1))
        self._commit((eng, v), reads, writes)

    def _dma(self, eng, chan, fn, reads=(), writes=()):
        if chan not in self.semnames:
            self.semnames.append(chan)
        self._deps(eng, reads, writes)
        v = self.count.get(chan, 0) + 16
        self.count[chan] = v
        self.streams[eng].append(('op', fn, chan, 16))
        self._commit((chan, v), reads, writes)

    def wait_all(self, eng, keys):
        self._deps(eng, keys, ())

    def emit(self):
        nc = self.nc
        needed = {}
        for e in self.ENG:
            for ent in self.streams[e]:
                if ent[0] == 'wait':
                    needed.setdefault(ent[1], set()).add(ent[2])
        remap = {}
        for sname, vals in needed.items():
            if sname in self.ENG:
                remap[sname] = {v: i + 1 for i, v in enumerate(sorted(vals))}
        with ExitStack() as st:
            sems = {s: st.enter_context(nc.semaphore("s_" + "".join(ch if ch.isalnum() else "_" for ch in str(s)))) for s in self.semnames}
            block = st.enter_context(nc.Block())
            engobj = {'pe': 'tensor', 'act': 'scalar', 'dve': 'vector', 'pool': 'gpsimd', 'sp': 'sync'}

            def mk(e):
                def body(eo):
                    idx = 0
                    rm = remap.get(e, {})
                    for ent in self.streams[e]:
                        if ent[0] == 'wait':
                            v = ent[2]
                            if ent[1] in remap:
                                v = remap[ent[1]][v]
                            eo.wait_ge(sems[ent[1]], v)
                        else:
                            ins = ent[1](eo)
                            if ent[2] == e:
                                idx += 1
                                if idx in rm:
                                    ins.then_inc(sems[e], 1)
                            else:
                                ins.then_inc(sems[ent[2]], ent[3])
                return body
            for e in self.ENG:
                getattr(block, engobj[e])(mk(e))


def _keys(*vs):
    ks = ()
    for v in vs:
        if isinstance(v, V):
            ks += v.keys
    return ks


def _a(v):
    return v.ap if isinstance(v, V) else v


class K:
    def __init__(self, S, depth, moe_layers, stages="kv,norm,rg,ml,xa,ffn"):
        self.S, self.depth, self.moe_layers = S, depth, moe_layers
        self.stages = set(stages.split(","))
        self.NT = S // T
        nc = self.nc = bass.Bass("TRN2", target_bir_lowering=False)
        self.P = Prog(nc)
        self.st = ExitStack()
        self.din = {}
        self.BANKS = {'m': [5, 6, 7], 'f': [0, 1, 2, 3, 4]}
        self.SLOTS = {'m': [5, 6, 7], 'f': [0, 1, 2, 3, 4]}
        self.bank_pos = {'m': 0, 'f': 0}
        self.slot_pos = {'m': 0, 'f': 0}
        self.cur_pool = 'm'
        self.stg_i = 0
        self.pids = {}
        self.marks = []
        self.leftover = []

    def tick(self):
        lst = getattr(self, 'feed_lst', None)
        if not lst:
            return
        self.ticks_left -= 1
        if self.feed_delay > 0:
            self.feed_delay -= 1
            return
        remaining = len(lst) - self.feed_pos
        if remaining <= 0:
            return
        fair = remaining / max(1, self.ticks_left)
        quota = int(2.0 * fair) + 2
        min_n = max(1, int(fair + 0.999))
        written = {}
        n = 0
        while self.feed_pos < len(lst) and n < quota:
            r = lst[self.feed_pos]
            eng = r[1]
            reads = r[3] if r[0] == 0 else r[4]
            writes = r[4] if r[0] == 0 else r[5]
            if n >= min_n and any(written.get(k, eng) != eng for k in reads):
                break
            self.feed_pos = self.P.feed(lst, self.feed_pos, 1)
            n += 1
            for k in writes:
                written[k] = eng

    def mm(self, out, lhsT, rhs, start=True, stop=True):
        self.P.op('pe', lambda e: e.matmul(out.ap, lhsT.ap, rhs.ap, start=start, stop=stop),
                  reads=_keys(lhsT, rhs), writes=out.keys)

    def tr(self, out, in_, ident):
        self.P.op('pe', lambda e: e.transpose(out.ap, in_.ap, ident.ap), reads=_keys(in_, ident), writes=out.keys)

    def act(self, out, in_, func, bias=0.0, scale=1.0, accum=None, eng='act'):
        kw = {}
        if accum is not None:
            kw['accum_out'] = accum.ap
        self.P.op(eng, lambda e: e.activation(out.ap, in_.ap, func, bias=_a(bias), scale=_a(scale), **kw),
                  reads=_keys(in_, bias, scale), writes=_keys(out, accum))

    def tt(self, out, a, b, op, eng='dve'):
        self.P.op(eng, lambda e: e.tensor_tensor(out.ap, a.ap, b.ap, op), reads=_keys(a, b), writes=out.keys)

    def ts(self, out, a, s1, s2, op0, op1=None, eng='dve'):
        if op1 is None:
            self.P.op(eng, lambda e: e.tensor_scalar(out.ap, a.ap, _a(s1), None, op0),
                      reads=_keys(a, s1), writes=out.keys)
        else:
            self.P.op(eng, lambda e: e.tensor_scalar(out.ap, a.ap, _a(s1), _a(s2), op0, op1),
                      reads=_keys(a, s1, s2), writes=out.keys)

    def stt(self, out, in0, scalar, in1, op0, op1, eng='dve'):
        self.P.op(eng, lambda e: e.scalar_tensor_tensor(out.ap, in0.ap, _a(scalar), in1.ap, op0, op1),
                  reads=_keys(in0, scalar, in1), writes=out.keys)

    def cp(self, out, in_, eng='dve'):
        if eng == 'act':
            self.P.op('act', lambda e: e.copy(out.ap, in_.ap), reads=in_.keys, writes=out.keys)
        else:
            self.P.op(eng, lambda e: e.tensor_copy(out.ap, in_.ap), reads=in_.keys, writes=out.keys)

    def red(self, out, in_, op, eng='dve'):
        self.P.op(eng, lambda e: e.tensor_reduce(out.ap, in_.ap, AX.X, op), reads=in_.keys, writes=out.keys)

    def recip(self, out, in_):
        self.P.op('dve', lambda e: e.reciprocal(out.ap, in_.ap), reads=in_.keys, writes=out.keys)

    def memset(self, out, val, eng='dve'):
        self.P.op(eng, lambda e: e.memset(out.ap, val), writes=out.keys)

    def scan(self, out, d0, d1, init, op0, op1):
        self.P.op('dve', lambda e: e.tensor_tensor_scan(out.ap, d0.ap, d1.ap, _a(init), op0, op1),
                  reads=_keys(d0, d1, init), writes=out.keys)

    def dma(self, eng, chan, out, in_):
        self.P.dma(eng, chan, lambda e: e.dma_start(out=out.ap, in_=in_.ap), reads=in_.keys, writes=out.keys)

    def sb(self, name, shape, dt, key=None):
        t = self.st.enter_context(self.nc.sbuf_tensor("sb_" + name, shape, dt))
        return V(t[:], (key or name,))

    def bank(self):
        pool = self.BANKS[self.cur_pool]
        i = self.bank_pos[self.cur_pool]
        self.bank_pos[self.cur_pool] = (i + 1) % len(pool)
        return self.banks[pool[i]]

    def dram_in(self, name, shape):
        ap = self.nc.dram_tensor(name, list(shape), F32, kind="ExternalInput").ap()
        self.din[name] = ap
        return ap

    def convert(self, name, src, nk, C):
        if name in self.pids:
            return
        n = nk * C
        pid = len(self.pids)
        self.pids[name] = pid
        if pid // 256 >= len(self.scrs):
            self.scrs.append(self.nc.dram_tensor("wscr%d" % len(self.scrs), [256, 128, SLOT], BF16, kind="Internal").ap())
        ch = pid % NCVT
        self.dma('pool', ('cvt', ch),
                 V(self.scrs[pid // 256][pid % 256, :, 0:n].rearrange("p (k c) -> p k c", k=nk), (('scr', pid), ('cvtch', ch))),
                 V(src.rearrange("(k p) c -> p k c", p=128), ()))

    def wpanel(self, name, src, nk, C):
        n = nk * C
        assert n <= SLOT
        pool = self.SLOTS[self.cur_pool]
        i = self.slot_pos[self.cur_pool]
        self.slot_pos[self.cur_pool] = (i + 1) % len(pool)
        s = pool[i]
        slot = V(self.wbf[s].ap[:, 0:n], (('wbf', s),))
        if name not in self.pids:
            self.convert(name, src, nk, C)
        pid = self.pids[name]
        self.dma('sp', ('wl16', s), slot, V(self.scrs[pid // 256][pid % 256, :, 0:n], (('scr', pid),)))
        return slot.re("p (k c) -> p k c", k=nk)

    def build(self):
        nc, P, S, depth = self.nc, self.P, self.S, self.depth
        n_moe = max(1, len(self.moe_layers))
        n_dense = max(1, depth - len(self.moe_layers))
        di = self.dram_in
        x_d = di("x", (S, D))
        mem_d = di("mem", (MEM, D))
        pp_d = di("pp", (128, PP_L * depth))
        bc_d = di("bc", (128, 1032 + 8 * depth))
        w_in = di("w_in", (depth, D, N_IN))
        rg_w_a = di("rg_w_a", (depth, 4, 256, 256))
        rg_w_x = di("rg_w_x", (depth, 4, 256, 256))
        ml_w_q = di("ml_w_q", (depth, 4, 256, 256))
        ml_w_k = di("ml_w_k", (depth, 4, 256, 256))
        ml_w_v = di("ml_w_v", (depth, 4, 256, 256))
        w_kv = di("w_kv", (depth, D, 2 * D))
        w_br = [di("w_br_rg", (depth, D, D)), di("w_br_ml", (depth, D, D)), di("w_br_xa", (depth, D, D))]
        w_out = di("w_out", (depth, D, D))
        ffn_w1 = di("ffn_w1", (n_dense, D, DFF))
        ffn_w3 = di("ffn_w3", (n_dense, D, DFF))
        ffn_w2 = di("ffn_w2", (n_dense, DFF, D))
        router_w = di("router_w", (n_moe, D, NEXP))
        moe_w1 = di("moe_w1", (n_moe, NEXP, D, DFF))
        moe_w3 = di("moe_w3", (n_moe, NEXP, D, DFF))
        moe_w2 = di("moe_w2", (n_moe, NEXP, DFF, D))
        y_d = nc.dram_tensor("y", [S, D], F32, kind="ExternalOutput").ap()
        npan = depth * 110 + (len(self.moe_layers) * 8 + n_dense) * 44 + 16
        self.scrs = []

        sb = self.sb
        self.banks = []
        for b in range(8):
            t = self.st.enter_context(nc.psum_tensor("ps%d" % b, [128, 512], F32))
            self.banks.append(V(t[:], (('ps', b),)))
        self.wbf = [sb("wbf%d" % i, [128, SLOT], BF16, ('wbf', i)) for i in range(NBF)]
        xres_l = [sb("xres%d" % p_, [128, NCH, D], F32) for p_ in range(2)]
        xr_l = [[V(xres_l[p_].ap[:, c, :], (('x', p_, c),)) for c in range(NCH)] for p_ in range(2)]
        cx = {'p': 0}
        hT_t = sb("hTm", [128, KC, T], BF16)
        hTf_t = sb("hTf", [128, KC, T], BF16)
        ybr_t = sb("ybr", [128, KC, T], BF16)
        ybr = [V(ybr_t.ap[:, k, :], (('ybr', k),)) for k in range(KC)]
        mgb_t = sb("mgb", [128, KC, T], BF16)
        mgb = [V(mgb_t.ap[:, k, :], (('mgb', k),)) for k in range(KC)]
        hid_t = sb("hid", [128, NF // 2, T], BF16)
        hid = [V(hid_t.ap[:, f, :], (('hid', f),)) for f in range(NF // 2)]
        WA = sb("WA", [128, 2, T + 4], F32)
        WB = sb("WB", [128, 2, T + 4], F32)
        WC = sb("WC", [128, 2, T + 4], F32)
        WD = sb("WD", [128, 2, T + 4], F32)
        WE = sb("WE", [128, 2, T + 4], F32)
        WFb = sb("WF", [128, 2, T + 4], F32)
        h32 = V(WA.ap.rearrange("p a b -> p (a b)")[:, 0:KC * 128].rearrange("p (k t) -> p k t", k=KC), WA.keys)
        xn = V(WB.ap.rearrange("p a b -> p (a b)")[:, 0:D], WB.keys)
        B0 = sb("B0", [128, 2, T], BF16)
        B1 = sb("B1", [128, 2, T], BF16)
        B2 = sb("B2", [128, 2, T], BF16)
        B3 = sb("B3", [128, 2, T], BF16)
        B4 = sb("B4", [128, NCH, 256], BF16)
        B5 = sb("B5", [128, NCH, 260], BF16)
        Cst = sb("Cst", [128, depth * 8, 257], F32)
        Cbf = sb("Cbf", [128, depth * 8, 257], BF16)
        Kf = sb("Kf", [128, depth * KC, MEM], BF16)
        Vt = sb("Vt", [128, depth * 2, D], BF16)
        memx = V(hid_t.ap.bitcast(F32).rearrange("p a b -> p (a b)")[:, 0:2 * D].rearrange("p (c d) -> p c d", c=2),
                 tuple(k for h_ in hid for k in h_.keys))
        pp = sb("pp", [128, PP_L * depth], F32)
        bcp = sb("bcp", [128, 1032 + 8 * depth], F32)
        id32 = sb("id32", [128, 128], F32)
        idbf = sb("idbf", [128, 128], BF16)
        tri = sb("tri", [128, 128], F32)
        ones = sb("ones", [128, 128], F32)
        maskT = sb("maskT", [128, 128], F32)
        halo = sb("halo", [128, depth * 2 * KC, 4], F32)
        hst = sb("hst", [128, depth * KC], F32)
        cA = sb("cA", [128, depth * KC], F32)
        cA2 = sb("cA2", [128, depth * KC], F32)
        sm = sb("sm", [128, 64], F32)
        gsc = sb("gsc", [128, 6, 16], F32)
        rw32 = sb("rw32", [128, KC, NEXP], F32)
        rgate = sb("rgate", [128, NCH, NEXP], F32)
        rtmp = sb("rtmp", [128, 4, NEXP], F32)
        ST = sb("ST", [128, 128], BF16)
        Vs = sb("Vs", [128, 260], BF16)
        VsL = sb("VsL", [128, 260], BF16)
        hm = sb("hm", [128, 256], F32)
        hnb = sb("hnb", [128, 256], BF16)
        ex = sb("ex", [128, 256], F32)
        pb = sb("pb", [128, 256], BF16)
        silt = sb("silt", [128, T], F32)

        self.dma('pool', 'c0', pp, V(pp_d, ()))
        self.dma('pool', 'c1', bcp, V(bc_d, ()))
        P.op('pool', lambda e: e.memset(ones.ap, 1.0), writes=ones.keys)
        P.op('pool', lambda e: e.affine_select(id32.ap, ones.ap, [[-1, 128]], ALU.is_equal, 0.0, base=0, channel_multiplier=1),
             reads=ones.keys, writes=id32.keys)
        P.op('pool', lambda e: e.affine_select(tri.ap, ones.ap, [[1, 128]], ALU.is_ge, 0.0, base=0, channel_multiplier=-1),
             reads=ones.keys, writes=tri.keys)
        self.cp(idbf, id32, eng='pool')
        self.cp(maskT, tri, eng='pool')
        self.memset(halo, 0.0, eng='pool')
        self.memset(hst, 0.0, eng='pool')
        self.memset(Cst, 0.0, eng='pool')
        self.memset(Cbf, 0.0, eng='pool')
        for l in range(depth):
            lam = pp[:, l * PP_L + PP_OFF["lam"]: l * PP_L + PP_OFF["lam"] + 8]
            c = cA[:, l * 8:(l + 1) * 8]
            c2 = cA2[:, l * 8:(l + 1) * 8]
            self.act(c, lam, AF.Exp, scale=-1.0)
            self.act(c, c, AF.Ln, bias=1.0)
            self.ts(c2, c, -16.0, None, ALU.mult)
            self.ts(c, c, -8.0, None, ALU.mult)
        for j, l in enumerate(self.moe_layers):
            assert j == 0
            self.dma('pool', 'c2', rw32, V(router_w[j].rearrange("(k p) e -> p k e", p=128), ()))

        def ppc(l, name, c, n=1):
            o = l * PP_L + PP_OFF[name] + c
            return pp[:, o:o + n]

        def norm_T(src_tc, gname, l, tc_idx, dst_hT, ncols, want32=False):
            ss = sm[:, 0:1]
            rs = sm[:, 1:2]
            self.act(xn, src_tc, AF.Square, accum=ss)
            self.act(rs, ss, AF.Sqrt, bias=EPS, scale=1.0 / D)
            self.recip(rs, rs)
            self.ts(xn, src_tc, rs, None, ALU.mult)
            for half in range(2):
                bk = self.bank()
                for q in range(4):
                    k = half * 4 + q
                    self.tr(bk[:, q * 128:(q + 1) * 128], xn[:, k * 128:(k + 1) * 128], id32)
                g = ppc(l, gname, half * 4, 4)
                gb = V(g.ap.unsqueeze(2).to_broadcast([128, 4, 128]), g.keys)
                src = bk.re("p (q t) -> p q t", q=4)
                self.tt(dst_hT[:, half * 4:half * 4 + 4, tc_idx * 128:(tc_idx + 1) * 128], src, gb, ALU.mult)
                if want32:
                    self.tt(h32[:, half * 4:half * 4 + 4, :], src, gb, ALU.mult, eng='dve')

        def mem_kv(l):
            self.dma('pool', 'memld', memx, V(mem_d.rearrange("(c p) d -> p c d", p=128), ()))
            memT = ybr_t
            for c in range(2):
                norm_T(memx[:, c, :], "gmem", l, c, V(memT.ap, tuple(k for y in ybr for k in y.keys)), 256)
            mT = [V(ybr_t.ap[:, k, 0:MEM], ybr[k].keys) for k in range(KC)]
            for pi in range(4):
                wp = self.wpanel(("wkvK", l, pi), w_kv[l][:, pi * 256:(pi + 1) * 256], KC, 256)
                for m2 in range(2):
                    mo = pi * 2 + m2
                    bk = self.bank()
                    for k in range(KC):
                        self.mm(bk[:, 0:MEM], wp[:, k, m2 * 128:(m2 + 1) * 128], mT[k], start=(k == 0), stop=(k == KC - 1))
                    self.cp(Kf[:, l * KC + mo, :], bk[:, 0:MEM], eng='act')
            for pi in range(4):
                wp = self.wpanel(("wkvV", l, pi), w_kv[l][:, D + pi * 256:D + (pi + 1) * 256], KC, 256)
                for mc in range(2):
                    bk = self.bank()
                    for k in range(KC):
                        self.mm(bk[:, 0:256], mT[k][:, mc * 128:(mc + 1) * 128], wp[:, k, :], start=(k == 0), stop=(k == KC - 1))
                    self.cp(Vt[:, l * 2 + mc, pi * 256:(pi + 1) * 256], bk[:, 0:256], eng='act')

        hT = [V(hT_t.ap[:, k, :], hT_t.keys) for k in range(KC)]

        def proj_fm(wp, m2, rhs_list, nk, bk, N=T, tk=False):
            for k in range(nk):
                self.mm(bk[:, 0:N], wp[:, k, m2 * 128:(m2 + 1) * 128], rhs_list[k], start=(k == 0), stop=(k == nk - 1))
            if tk:
                self.tick()

        def conv_group(l, which, g, src_banks, dst, hidx0):
            wn, bn = ("crw", "crb") if which == 0 else ("cmw", "cmb")
            for m in range(2):
                ch = g * 2 + m
                hx = halo[:, (l * 2 + which) * KC + ch, 0:3]
                self.cp(WA[:, m, 0:3], hx, eng='dve')
                self.cp(WA[:, m, 3:3 + T], src_banks[m], eng='act')
                self.cp(hx, WA[:, m, T:T + 3], eng='dve')
                o = dst[:, m, 0:T]
                self.ts(o, WA[:, m, 0:T], ppc(l, wn, 0 * 8 + ch), ppc(l, bn, ch), ALU.mult, ALU.add)
                for j in range(1, 4):
                    self.stt(o, WA[:, m, j:j + T], ppc(l, wn, j * 8 + ch), o, ALU.mult, ALU.add)

        def merge_branch(l, b, ysrc):
            for pi in range(4):
                wpb = self.wpanel(("wbr", l, b, pi), w_br[b][l][:, pi * 256:(pi + 1) * 256], KC, 256)
                wpg = self.wpanel(("wg", l, b, pi), w_in[l][:, 5128 + b * D + pi * 256: 5128 + b * D + (pi + 1) * 256], KC, 256)
                for m2 in range(2):
                    mo = pi * 2 + m2
                    bg = self.bank()
                    proj_fm(wpg, m2, hT, KC, bg)
                    bp = self.bank()
                    proj_fm(wpb, m2, ysrc, KC, bp)
                    gt = WB[:, 0, 0:T]
                    self.act(gt, bg, AF.Sigmoid, bias=ppc(l, "bmg", b * 8 + mo))
                    self.tt(mgb[mo], bp, gt, ALU.mult)
            for pi in range(4):
                wpo = self.wpanel(("wout", l, pi), w_out[l][:, pi * 256:(pi + 1) * 256], KC, 256)
                for tc in range(NCH):
                    bk = self.bank()
                    for k in range(KC):
                        self.mm(bk[:, 0:256], mgb[k][:, tc * 128:(tc + 1) * 128], wpo[:, k, :], start=(k == 0), stop=(k == KC - 1))
                    xs = xr_l[cx['p']][tc][:, pi * 256:(pi + 1) * 256]
                    self.tt(xs, bk[:, 0:256], xs, ALU.add)

        def rg_branch(l):
            for g in range(4):
                wp = self.wpanel(("ax", l, g), w_in[l][:, g * 256:(g + 1) * 256], KC, 256)
                bks = []
                for m in range(2):
                    bk = self.bank()
                    proj_fm(wp, m, hT, KC, bk)
                    bks.append(bk)
                conv_group(l, 0, g, bks, WB, None)
                xcb = B0
                self.cp(xcb[:, :, :], WB[:, :, 0:T], eng='dve')
                wa = self.wpanel(("rga", l, g), rg_w_a[l, g], 2, 256)
                wx = self.wpanel(("rgx", l, g), rg_w_x[l, g], 2, 256)
                xcl = [xcb[:, 0, :], xcb[:, 1, :]]
                for m in range(2):
                    ch = g * 2 + m
                    br_ = self.bank()
                    proj_fm(wa, m, xcl, 2, br_)
                    bi_ = self.bank()
                    proj_fm(wx, m, xcl, 2, bi_)
                    r = WC[:, m, 0:T]
                    self.act(r, br_, AF.Sigmoid, bias=ppc(l, "rba", ch))
                    iv = WFb[:, m, 0:T]
                    self.act(iv, bi_, AF.Sigmoid, bias=ppc(l, "rbx", ch))
                    a = WD[:, m, 0:T]
                    self.act(a, r, AF.Exp, scale=cA[:, l * 8 + ch:l * 8 + ch + 1])
                    s = WE[:, m, 0:T]
                    self.act(s, r, AF.Exp, scale=cA2[:, l * 8 + ch:l * 8 + ch + 1])
                    self.act(s, s, AF.Sqrt, bias=1.0, scale=-1.0)
                    self.tt(s, s, iv, ALU.mult)
                    self.tt(s, s, WB[:, m, 0:T], ALU.mult)
                    hs = WC[:, m, 0:T]
                    hcol = hst[:, l * 8 + ch:l * 8 + ch + 1]
                    self.scan(hs, a, s, hcol, ALU.mult, ALU.add)
                    self.cp(hcol, hs[:, T - 1:T], eng='pool')
                wy = self.wpanel(("ay", l, g), w_in[l][:, D + g * 256: D + (g + 1) * 256], KC, 256)
                for m in range(2):
                    ch = g * 2 + m
                    bk = self.bank()
                    proj_fm(wy, m, hT, KC, bk)
                    ay = WD[:, m, 0:T]
                    self.cp(ay, bk, eng='act')
                    u = WE[:, m, 0:T]
                    self.act(u, ay, AF.Square)
                    self.ts(u, u, 0.044715, 1.0, ALU.mult, ALU.add)
                    self.tt(u, u, ay, ALU.mult)
                    self.act(u, u, AF.Sigmoid, scale=1.5957691216057308)
                    self.tt(u, u, ay, ALU.mult, eng='pool')
                    self.tt(ybr[ch], u, WC[:, m, 0:T], ALU.mult, eng='pool')
            self.marks.append((-1, l, 'rg_merge', dict(P.count)))
            merge_branch(l, 0, ybr)

        def ml_gates(l):
            wp = self.wpanel(("wif", l), w_in[l][:, 4096:4104], KC, 8)
            bk = self.bank()
            for c in range(NCH):
                for k in range(KC):
                    self.mm(bk[:, c * 8:(c + 1) * 8], hT[k][:, c * 128:(c + 1) * 128], wp[:, k, :], start=(k == 0), stop=(k == KC - 1))
            pre = bk[:, 0:NCH * 8].re("p (c e) -> p c e", c=NCH)
            bif = bcp[:, BC_BI + l * 8: BC_BI + l * 8 + 8]
            ig = gsc[:, 4, :].re("p (c h) -> p c h", c=NCH)
            fg = gsc[:, 5, :].re("p (c h) -> p c h", c=NCH)
            bi_b = V(bif.ap[:, 0:4].unsqueeze(1).to_broadcast([128, NCH, 4]), bif.keys)
            bf_b = V(bif.ap[:, 4:8].unsqueeze(1).to_broadcast([128, NCH, 4]), bif.keys)
            self.tt(ig, pre[:, :, 0:4], bi_b, ALU.add)
            self.tt(fg, pre[:, :, 4:8], bf_b, ALU.add)
            sp = gsc[:, 5, :]
            self.act(sp, sp, AF.Exp, scale=-1.0)
            self.act(sp, sp, AF.Ln, bias=1.0)
            b2 = self.bank()
            self.mm(b2[:, 0:16], tri, sp)
            self.mm(b2[:, 16:32], ones, sp)
            self.tt(gsc[:, 0, :], gsc[:, 4, :], b2[:, 0:16], ALU.add)
            self.act(gsc[:, 0, :], gsc[:, 0, :], AF.Exp)
            self.act(gsc[:, 2, :], b2[:, 0:16], AF.Exp)
            self.act(gsc[:, 3, :], b2[:, 16:32], AF.Exp, scale=-1.0)
            self.tt(gsc[:, 1, :], gsc[:, 0, :], gsc[:, 3, :], ALU.mult)

        def ml_branch(l):
            ml_gates(l)
            for g in range(4):
                wp = self.wpanel(("mu", l, g), w_in[l][:, 2048 + g * 256: 2048 + (g + 1) * 256], KC, 256)
                bks = []
                for m in range(2):
                    bk = self.bank()
                    proj_fm(wp, m, hT, KC, bk)
                    bks.append(bk)
                ub = B1
                for m in range(2):
                    self.cp(ub[:, m, :], bks[m], eng='dve')
                conv_group(l, 1, g, bks, WB, None)
                cb = B0
                self.act(cb[:, :, :], WB[:, :, 0:T], AF.Silu)
                cbl = [cb[:, 0, :], cb[:, 1, :]]
                qf, kf, ktok, vaug = B2, B3, B4, B5
                self.memset(vaug, 1.0, eng='pool')
                wq = self.wpanel(("mq", l, g), ml_w_q[l, g], 2, 256)
                for m in range(2):
                    bk = self.bank()
                    proj_fm(wq, m, cbl, 2, bk)
                    self.cp(qf[:, m, :], bk, eng='act')
                wk = self.wpanel(("mk", l, g), ml_w_k[l, g], 2, 256)
                for m in range(2):
                    bk = self.bank()
                    proj_fm(wk, m, cbl, 2, bk)
                    self.act(kf[:, m, :], bk, AF.Copy, scale=1.0 / 16.0)
                for c in range(NCH):
                    cs = slice(c * 128, (c + 1) * 128)
                    bk = self.bank()
                    for m in range(2):
                        self.mm(bk[:, 0:256], cb[:, m, cs], wk[:, m, :], start=(m == 0), stop=(m == 1))
                    self.act(ktok[:, c, :], bk[:, 0:256], AF.Copy, scale=1.0 / 16.0)
                wv = self.wpanel(("mv", l, g), ml_w_v[l, g], 2, 256)
                for c in range(NCH):
                    cs = slice(c * 128, (c + 1) * 128)
                    bk = self.bank()
                    for m in range(2):
                        self.mm(bk[:, 0:256], ub[:, m, cs], wv[:, m, :], start=(m == 0), stop=(m == 1))
                    self.cp(vaug[:, c, 0:256], bk[:, 0:256], eng='dve')
                wo = self.wpanel(("mo", l, g), w_in[l][:, 3072 + g * 256: 3072 + (g + 1) * 256], KC, 256)
                otok = WD
                ot = V(WD.ap.rearrange("p a b -> p (a b)")[:, 0:NCH * 256].rearrange("p (c d) -> p c d", c=NCH), WD.keys)
                for c in range(NCH):
                    bk = self.bank()
                    for k in range(KC):
                        self.mm(bk[:, 0:256], hT[k][:, c * 128:(c + 1) * 128], wo[:, k, :], start=(k == 0), stop=(k == KC - 1))
                    self.act(ot[:, c, :], bk[:, 0:256], AF.Sigmoid)
                self.marks.append((-1, l, 'ml_chunks%d' % g, dict(P.count)))
                for c in range(NCH):
                    cs = slice(c * 128, (c + 1) * 128)
                    col = c * 4 + g
                    ec = gsc[:, 0, col:col + 1]
                    ecL = gsc[:, 1, col:col + 1]
                    enb = gsc[:, 2, col:col + 1]
                    ebL = gsc[:, 3, col:col + 1]
                    bs = self.bank()
                    for m in range(2):
                        self.mm(bs[:, 0:128], kf[:, m, cs], qf[:, m, cs], start=(m == 0), stop=(m == 1))
                    self.tt(ST, bs[:, 0:128], maskT, ALU.mult)
                    self.ts(Vs[:, 0:257], vaug[:, c, 0:257], ec, None, ALU.mult)
                    self.act(VsL[:, 0:257], vaug[:, c, 0:257], AF.Copy, scale=ecL)
                    bn = self.bank()
                    for m in range(2):
                        self.mm(bn[:, 0:257], qf[:, m, cs], Cbf[:, l * 8 + g * 2 + m, :], start=(m == 0), stop=False)
                    self.mm(bn[:, 0:257], ST, Vs[:, 0:257], start=False, stop=True)
                    den = sm[:, 8:9]
                    self.ts(den, bn[:, 256:257], -1.0, None, ALU.mult)
                    self.tt(den, den, bn[:, 256:257], ALU.max)
                    self.tt(den, den, enb, ALU.max)
                    self.recip(den, den)
                    self.stt(hm, bn[:, 0:256], den, ot[:, c, :], ALU.mult, ALU.mult)
                    ss = sm[:, 9:10]
                    self.act(ex, hm, AF.Square, accum=ss)
                    self.act(ss, ss, AF.Sqrt, bias=EPS, scale=1.0 / 256.0)
                    self.recip(ss, ss)
                    self.ts(hnb, hm, ss, None, ALU.mult)
                    bt = self.bank()
                    btb = bt.cast(BF16)
                    for m in range(2):
                        self.tr(btb[:, m * 128:(m + 1) * 128], hnb[:, m * 128:(m + 1) * 128], idbf)
                    for m in range(2):
                        self.ts(ybr[g * 2 + m][:, cs], btb[:, m * 128:(m + 1) * 128], ppc(l, "gml", g * 2 + m), None, ALU.mult)
                    for m in range(2):
                        bd = self.bank()
                        self.mm(bd[:, 0:257], ktok[:, c, m * 128:(m + 1) * 128], VsL[:, 0:257])
                        Cm = Cst[:, l * 8 + g * 2 + m, :]
                        self.stt(Cm, Cm, ebL, bd[:, 0:257], ALU.mult, ALU.add)
                        self.cp(Cbf[:, l * 8 + g * 2 + m, :], Cm, eng='act')
            self.marks.append((-1, l, 'ml_merge', dict(P.count)))
            merge_branch(l, 1, ybr)

        def xa_branch(l):
            for g in range(4):
                wp = self.wpanel(("xq", l, g), w_in[l][:, 4104 + g * 256: 4104 + (g + 1) * 256], KC, 256)
                qf = B2
                for m in range(2):
                    bk = self.bank()
                    proj_fm(wp, m, hT, KC, bk)
                    self.act(qf[:, m, :], bk, AF.Copy, scale=1.0 / 16.0)
                PT = B1
                for c in range(NCH):
                    cs = slice(c * 128, (c + 1) * 128)
                    bk = self.bank()
                    for m in range(2):
                        self.mm(bk[:, 0:MEM], qf[:, m, cs], Kf[:, l * KC + g * 2 + m, :], start=(m == 0), stop=(m == 1))
                    mx = sm[:, 12:13]
                    self.red(mx, bk[:, 0:MEM], ALU.max)
                    self.ts(mx, mx, -1.0, None, ALU.mult)
                    ssum = sm[:, 13:14]
                    self.act(ex, bk[:, 0:MEM], AF.Exp, bias=mx, accum=ssum)
                    self.recip(ssum, ssum)
                    self.ts(pb, ex, ssum, None, ALU.mult)
                    bt = self.bank()
                    btb = bt.cast(BF16)
                    for mc in range(2):
                        self.tr(btb[:, mc * 128:(mc + 1) * 128], pb[:, mc * 128:(mc + 1) * 128], idbf)
                    self.cp(PT[:, :, cs], btb[:, 0:256].re("p (a t) -> p a t", a=2), eng='act')
                for m2 in range(2):
                    bk = self.bank()
                    for mc in range(2):
                        self.mm(bk, Vt[:, l * 2 + mc, g * 256 + m2 * 128: g * 256 + (m2 + 1) * 128], PT[:, mc, :],
                                start=(mc == 0), stop=(mc == 1))
                    self.cp(ybr[g * 2 + m2], bk, eng='act')
            self.marks.append((-1, l, 'xa_merge', dict(P.count)))
            merge_branch(l, 2, ybr)

        def ffn(w1, w3, w2, name, hsrc, gate_e=None):
            nf_h = NF // 2
            for half in range(2):
                f0 = half * nf_h
                for pj in range(nf_h // 2):
                    c0 = (f0 + pj * 2) * 128
                    pa = self.wpanel((name, "w1", half, pj), w1[:, c0:c0 + 256], KC, 256)
                    pb3 = self.wpanel((name, "w3", half, pj), w3[:, c0:c0 + 256], KC, 256)
                    for m2 in range(2):
                        up_chunk(pa, pb3, m2, hid[pj * 2 + m2], hsrc)
                for ch in range(2):
                    accs = [self.bank() for _ in range(NCH)]
                    for fp in range(0, nf_h, 4):
                        nfc = min(4, nf_h - fp)
                        r0 = (f0 + fp) * 128
                        wp = self.wpanel((name, "w2", half, ch, fp), w2[r0:r0 + nfc * 128, ch * 512:(ch + 1) * 512], nfc, 512)
                        for tc in range(NCH):
                            for f in range(nfc):
                                fi = fp + f
                                self.mm(accs[tc], hid[fi][:, tc * 128:(tc + 1) * 128], wp[:, f, :],
                                        start=(fi == 0), stop=(fi == nf_h - 1))
                                if f % 2 == 1:
                                    self.tick()
                    for tc in range(NCH):
                        xs = xr_l[cx['p']][tc][:, ch * 512:(ch + 1) * 512]
                        if gate_e is None:
                            self.tt(xs, accs[tc], xs, ALU.add)
                        else:
                            self.stt(xs, accs[tc], rgate[:, tc, gate_e:gate_e + 1], xs, ALU.mult, ALU.add)

        def ffn_convert(w1, w3, w2, name):
            nf_h = NF // 2
            for half in range(2):
                f0 = half * nf_h
                for pj in range(nf_h // 2):
                    c0 = (f0 + pj * 2) * 128
                    self.convert((name, "w1", half, pj), w1[:, c0:c0 + 256], KC, 256)
                    self.convert((name, "w3", half, pj), w3[:, c0:c0 + 256], KC, 256)
                for ch in range(2):
                    for fp in range(0, nf_h, 4):
                        nfc = min(4, nf_h - fp)
                        r0 = (f0 + fp) * 128
                        self.convert((name, "w2", half, ch, fp), w2[r0:r0 + nfc * 128, ch * 512:(ch + 1) * 512], nfc, 512)

        def up_chunk(pa, pb3, m2, hdst, hsrc):
            b1 = self.bank()
            proj_fm(pa, m2, hsrc, KC, b1, tk=True)
            b3 = self.bank()
            proj_fm(pb3, m2, hsrc, KC, b3, tk=True)
            s = silt
            self.act(s, b1, AF.Silu)
            self.tt(hdst, b3, s, ALU.mult)

        def router(l, tc):
            bk = self.bank()
            for k in range(KC):
                self.mm(bk[:, 0:NEXP], h32[:, k, :], rw32[:, k, :], start=(k == 0), stop=(k == KC - 1))
            lg = rtmp[:, 0, :]
            self.tt(lg, bk[:, 0:NEXP], bcp[:, BC_RB:BC_RB + NEXP], ALU.add)
            m1 = sm[:, 16:17]
            self.red(m1, lg, ALU.max)
            eq = rtmp[:, 1, :]
            self.ts(eq, lg, m1, None, ALU.is_equal)
            l2 = rtmp[:, 2, :]
            self.stt(l2, eq, -1e30, lg, ALU.mult, ALU.add)
            m2 = sm[:, 17:18]
            self.red(m2, l2, ALU.max)
            sel = rtmp[:, 1, :]
            self.ts(sel, lg, m2, None, ALU.is_ge)
            nm1 = sm[:, 18:19]
            self.ts(nm1, m1, -1.0, None, ALU.mult)
            e = rtmp[:, 3, :]
            self.act(e, lg, AF.Exp, bias=nm1)
            self.tt(e, e, sel, ALU.mult)
            sume = sm[:, 19:20]
            self.red(sume, e, ALU.add)
            self.recip(sume, sume)
            self.ts(rgate[:, tc, :], e, sume, None, ALU.mult)

        hT_all = V(hT_t.ap, hT_t.keys)
        hTf = [V(hTf_t.ap[:, k, :], hTf_t.keys) for k in range(KC)]
        hTf_all = V(hTf_t.ap, hTf_t.keys)
        NT = self.NT
        assert depth == 2 and self.moe_layers == [1]

        def allx(p_):
            return V(xres_l[p_].ap, xres_l[p_].keys + tuple(k for c in range(NCH) for k in xr_l[p_][c].keys))

        def xtc(p_, tc):
            return V(xres_l[p_].ap[:, tc, :], xr_l[p_][tc].keys + xres_l[p_].keys)

        def load_x(t):
            p_ = t % 2
            self.dma('pool', 'xld', allx(p_), V(x_d[t * T:(t + 1) * T, :].rearrange("(c p) d -> p c d", p=128), ()))

        def mixer(t, l):
            cx['p'] = t % 2
            self.cur_pool = 'm'
            if t == 0:
                mem_kv(l)
            for tc in range(NCH):
                norm_T(xtc(t % 2, tc), "g_mix", l, tc, hT_all, T)
            rg_branch(l)
            ml_branch(l)
            xa_branch(l)

        def dense_ffn(t, l):
            cx['p'] = t % 2
            self.cur_pool = 'f'
            for tc in range(NCH):
                norm_T(xtc(t % 2, tc), "g_ffn", l, tc, hT_all, T)
            j = l // 2
            ffn(ffn_w1[j], ffn_w3[j], ffn_w2[j], ("ffn", l), hT)

        def moe_pre(t, l):
            cx['p'] = t % 2
            self.cur_pool = 'f'
            for tc in range(NCH):
                norm_T(xtc(t % 2, tc), "g_ffn", l, tc, hTf_all, T, want32=True)
                router(l, tc)

        def final_store(t):
            p_ = t % 2
            self.cur_pool = 'f'
            for tc in range(NCH):
                xs = xtc(p_, tc)
                ss = sm[:, 0:1]
                rs = sm[:, 1:2]
                self.act(xn, xs, AF.Square, accum=ss)
                self.act(rs, ss, AF.Sqrt, bias=EPS, scale=1.0 / D)
                self.recip(rs, rs)
                self.stt(xs, xs, rs, bcp[:, BC_FINAL:BC_FINAL + D], ALU.mult, ALU.mult)
            self.dma('pool', 'yst', V(y_d[t * T:(t + 1) * T, :].rearrange("(c p) d -> p c d", p=128), ('y',)), allx(p_))

        def record(fn):
            P.begin_defer()
            fn()
            return P.end_defer()

        def experts(t, l, es, seg, delay=0):
            cx['p'] = t % 2
            self.cur_pool = 'f'
            n_units = len(es) * 2 * 84
            if seg and 'nodefer' in dbg:
                P.feed(seg, 0, len(seg))
                seg = None
            self.feed_lst = seg
            self.feed_pos = 0
            self.feed_delay = delay
            self.ticks_left = n_units - 8
            if seg and delay > 0:
                self.feed_pos = P.feed(seg, 0, 1)
            j = self.moe_layers.index(l)
            for e_ in es:
                ffn(moe_w1[j, e_], moe_w3[j, e_], moe_w2[j, e_], ("moe", l, e_), hTf, gate_e=e_)
            if seg:
                self.leftover.append(len(seg) - self.feed_pos)
                self.feed_pos = P.feed(seg, self.feed_pos, len(seg))
            self.feed_lst = None

        import os
        dbg = os.environ.get("KDBG", "")

        def startup():
            load_x(0)
            mixer(0, 0)
            dense_ffn(0, 0)
            mixer(0, 1)
            moe_pre(0, 1)

        def all_moe_convert():
            for e_ in range(NEXP):
                ffn_convert(moe_w1[0, e_], moe_w3[0, e_], moe_w2[0, e_], ("moe", 1, e_))

        if 'noprecvt' in dbg:
            startup()
        else:
            segS = record(startup)
            segC = record(all_moe_convert)
            LEAD = 12
            is_cvt = [(r[0] == 1 and isinstance(r[2], tuple) and r[2][0] == 'cvt') for r in segS]
            cvt_idx = [i for i, c in enumerate(is_cvt) if c]
            fed_c = 0
            seen_c = 0
            pc_ = 0
            ratio = max(1, (len(segS) - len(cvt_idx)) // max(1, len(segC)))
            cnt_ = 0
            for i, r in enumerate(segS):
                if is_cvt[i]:
                    seen_c += 1
                    continue
                while fed_c < min(len(cvt_idx), seen_c + LEAD):
                    P.feed(segS, cvt_idx[fed_c], 1)
                    fed_c += 1
                P.feed(segS, i, 1)
                cnt_ += 1
                if cnt_ % ratio == 0:
                    pc_ = P.feed(segC, pc_, 1)
            while fed_c < len(cvt_idx):
                P.feed(segS, cvt_idx[fed_c], 1)
                fed_c += 1
            pc_ = P.feed(segC, pc_, len(segC))
        for t in range(NT):
            if t + 1 < NT:
                segA = record(lambda: (load_x(t + 1), mixer(t + 1, 0)))
                experts(t, 1, [0, 1, 2, 3], segA, delay=30)
                dense_ffn(t + 1, 0)
                segB = record(lambda: mixer(t + 1, 1))
                experts(t, 1, [4, 5, 6, 7], segB)
            else:
                experts(t, 1, list(range(NEXP)), None)
            final_store(t)
            if t + 1 < NT:
                moe_pre(t + 1, 1)
        P.wait_all('pool', ['y'])
        P.emit()
        self.st.close()
        return nc


def pack_params(inp, depth):
    def col(a):
        return np.ascontiguousarray(np.asarray(a, np.float32).reshape(-1, 128).T)
    pp = np.zeros((128, PP_L * depth), np.float32)
    for l in range(depth):
        o = l * PP_L
        def put(name, arr):
            a = PP_OFF[name]
            pp[:, o + a:o + a + arr.shape[1]] = arr
        put("g_mix", col(inp["norm_mix_g"][l]))
        put("g_ffn", col(inp["norm_ffn_g"][l]))
        put("crw", np.concatenate([col(inp["conv_rg_w"][l][j]) for j in range(4)], axis=1))
        put("crb", col(inp["conv_rg_b"][l]))
        put("rba", col(inp["rg_b_a"][l]))
        put("rbx", col(inp["rg_b_x"][l]))
        put("lam", col(inp["rg_lambda"][l]))
        put("cmw", np.concatenate([col(inp["conv_ml_w"][l][j]) for j in range(4)], axis=1))
        put("cmb", col(inp["conv_ml_b"][l]))
        put("gml", col(inp["ml_norm_g"][l]))
        put("gmem", col(inp["mem_norm_g"][l]))
        put("bmg", col(inp["b_merge"][l]))
    bc = np.zeros((1032 + 8 * depth,), np.float32)
    bc[0:1024] = np.asarray(inp["final_norm_g"], np.float32)
    bc[1024:1032] = np.asarray(inp["router_b"], np.float32).reshape(-1)[:8]
    for l in range(depth):
        bc[1032 + l * 8:1032 + l * 8 + 4] = np.asarray(inp["ml_b_i"][l], np.float32)
        bc[1032 + l * 8 + 4:1032 + l * 8 + 8] = np.asarray(inp["ml_b_f"][l], np.float32)
    bcr = np.ascontiguousarray(np.broadcast_to(bc[None, :], (128, bc.shape[0])))
    return pp, bcr


W_NAMES = ["w_in", "rg_w_a", "rg_w_x", "ml_w_q", "ml_w_k", "ml_w_v", "w_kv", "w_br_rg", "w_br_ml", "w_br_xa",
           "w_out", "ffn_w1", "ffn_w3", "ffn_w2", "router_w", "moe_w1", "moe_w3", "moe_w2"]

_CACHE = {}


def run(inp, S, depth, moe_layers, n_cores, stages="kv,norm,rg,ml,xa,ffn"):
    key = (S, depth, tuple(moe_layers), stages)
    if key not in _CACHE:
        _CACHE[key] = K(S, depth, list(moe_layers), stages).build()
    nc = _CACHE[key]
    pp, bcr = pack_params(inp, depth)
    n_moe = max(1, len(moe_layers))
    n_dense = max(1, depth - len(moe_layers))

    def lead(n):
        return n_dense if n.startswith("ffn_") else (n_moe if (n.startswith("moe_") or n == "router_w") else depth)
    shared = {n: np.ascontiguousarray(np.asarray(inp[n], np.float32)[:lead(n)]) for n in W_NAMES}
    shared["pp"] = pp
    shared["bc"] = bcr
    in_maps = []
    for c in range(n_cores):
        m = dict(shared)
        m["x"] = np.ascontiguousarray(np.asarray(inp["x"][c], np.float32))
        m["mem"] = np.ascontiguousarray(np.asarray(inp["mem"][c], np.float32))
        in_maps.append(m)
    res = run_bass_kernel_spmd(nc, in_maps, core_ids=list(range(n_cores)))
    return np.stack([np.asarray(r["y"]) for r in res.results], axis=0)


def kernel(**inputs):
    return run(inputs, 4096, 2, (1,), 8).astype(np.float32)
```
